# Optimizing a Trainium2 kernel written in Bass

```python
import jax, jax.numpy as jnp
from jax import lax
import numpy as np

D_MODEL = 2048
BATCH = 1
SEQ = 8192
DEPTH = 1

GRID_W = 64
Q_BLOCK = 128
N_HEADS = 16
N_KV_HEADS = 4
HEAD_DIM = D_MODEL // N_HEADS
D_ATTN = N_HEADS * HEAD_DIM
D_KV = N_KV_HEADS * HEAD_DIM
ROPE_THETA = 10000.0
D_CONV = D_MODEL
CONV_GROUPS = 16
CONV_WIDTH = 3
N_GROUPS = 4
EXPERTS_PER_GROUP = 4
TOP_K = 2
D_FF_EXPERT = D_MODEL // 2
PLE_DIM = 256
LN_EPS = 1e-5
QK_EPS = 1e-6
DEEPNORM_ALPHA = (2 * DEPTH) ** 0.25
DEEPNORM_BETA = (8 * DEPTH) ** -0.25
IN_SIZES = (D_ATTN, D_KV, D_KV, D_CONV, D_CONV, D_CONV, D_MODEL, D_MODEL)
D_IN = D_ATTN + 2 * D_KV + 3 * D_CONV + 2 * D_MODEL

kernel_name = "hybrid_gqa_shortconv_hmoe_deepnorm"


def _split_points():
    return [int(v) for v in np.cumsum(IN_SIZES)[:-1]]


def layer_norm(x, g, b):
    xf = x.astype(jnp.float32)
    mu = jnp.mean(xf, axis=-1, keepdims=True)
    xc = xf - mu
    var = jnp.mean(xc * xc, axis=-1, keepdims=True)
    return (xc * lax.rsqrt(var + LN_EPS) * g.astype(jnp.float32) + b.astype(jnp.float32)).astype(x.dtype)


def head_rms_norm(t, g):
    tf = t.astype(jnp.float32)
    tf = tf * lax.rsqrt(jnp.mean(tf * tf, axis=-1, keepdims=True) + QK_EPS) * g.astype(jnp.float32)
    return tf.astype(t.dtype)


def axial_angles(seq_len):
    rows = seq_len // GRID_W
    row_idx = jnp.repeat(jnp.arange(rows, dtype=jnp.int32), GRID_W)
    col_idx = jnp.tile(jnp.arange(GRID_W, dtype=jnp.int32), rows)
    half = HEAD_DIM // 2
    inv_freq = 1.0 / (ROPE_THETA ** (jnp.arange(0, half, 2, dtype=jnp.float32) / half))
    ang_r = row_idx.astype(jnp.float32)[:, None] * inv_freq[None, :]
    ang_c = col_idx.astype(jnp.float32)[:, None] * inv_freq[None, :]
    return ang_r, ang_c


def _rotate(xh, ang):
    x1, x2 = jnp.split(xh, 2, axis=-1)
    cos = jnp.cos(ang)[None, :, None, :]
    sin = jnp.sin(ang)[None, :, None, :]
    return jnp.concatenate([x1 * cos - x2 * sin, x2 * cos + x1 * sin], axis=-1)


def axial_rope(t, ang_r, ang_c):
    tf = t.astype(jnp.float32)
    half = HEAD_DIM // 2
    out = jnp.concatenate([_rotate(tf[..., :half], ang_r), _rotate(tf[..., half:], ang_c)], axis=-1)
    return out.astype(t.dtype)


def bidir_gqa(q, k, v):
    b, s, _, _ = q.shape
    grp = N_HEADS // N_KV_HEADS
    n_blk = s // Q_BLOCK
    scale = HEAD_DIM ** -0.5
    qb = q.reshape(b, n_blk, Q_BLOCK, N_KV_HEADS, grp, HEAD_DIM).transpose(1, 0, 2, 3, 4, 5)

    def one_block(qi):
        sc = jnp.einsum('bqkgd,bskd->bkgqs', qi, k).astype(jnp.float32) * scale
        w = jax.nn.softmax(sc, axis=-1).astype(v.dtype)
        return jnp.einsum('bkgqs,bskd->bqkgd', w, v)

    o = lax.map(one_block, qb)
    return o.transpose(1, 0, 2, 3, 4, 5).reshape(b, s, N_HEADS * HEAD_DIM)


def short_conv_centred(u, w):
    up = jnp.pad(u, ((0, 0), (1, 1), (0, 0)))
    return up[:, :-2] * w[0] + up[:, 1:-1] * w[1] + up[:, 2:] * w[2]


def hierarchical_moe(xt, w_gr, b_gr, w_er, b_er, w1, w3, w2):
    gl = jnp.matmul(xt, w_gr).astype(jnp.float32) + b_gr.astype(jnp.float32)
    gp = jax.nn.softmax(gl, axis=-1)
    gp_top, g_idx = lax.top_k(gp, 1)
    oh_g = jax.nn.one_hot(g_idx[:, 0], N_GROUPS, dtype=jnp.float32)
    el = jnp.einsum('td,dge->tge', xt, w_er).astype(jnp.float32) + b_er.astype(jnp.float32)
    el_sel = jnp.einsum('tge,tg->te', el, oh_g)
    ep = jax.nn.softmax(el_sel, axis=-1)
    ew, e_idx = lax.top_k(ep, TOP_K)
    ew = ew / jnp.sum(ew, axis=-1, keepdims=True)
    e_w = jnp.einsum('tke,tk->te', jax.nn.one_hot(e_idx, EXPERTS_PER_GROUP, dtype=jnp.float32), ew)
    comb = ((gp_top[:, 0:1] * oh_g)[:, :, None] * e_w[:, None, :]).astype(xt.dtype)
    y = jnp.zeros_like(xt)
    for g in range(N_GROUPS):
        hg = jnp.einsum('td,edf->tef', xt, w1[g])
        hu = jnp.einsum('td,edf->tef', xt, w3[g])
        h = jax.nn.silu(hg) * hu * comb[:, g, :, None]
        y = y + jnp.einsum('tef,efd->td', h, w2[g])
    return y


def setup_inputs(seed: int = 0) -> dict:
    key = jax.random.key(seed)
    ks = jax.random.split(key, 32)
    L, D = DEPTH, D_MODEL
    G, E, F = N_GROUPS, EXPERTS_PER_GROUP, D_FF_EXPERT
    nrm = jax.random.normal
    f32 = jnp.float32
    x = nrm(ks[0], (BATCH, SEQ, D), f32)
    p = nrm(ks[1], (L, BATCH, SEQ, PLE_DIM), f32)
    emb_ln_g = 1.0 + 0.02 * nrm(ks[2], (D,), f32)
    emb_ln_b = 0.02 * nrm(ks[3], (D,), f32)
    v_lo = D_ATTN + D_KV
    col_scale = jnp.ones((D_IN,), f32).at[v_lo:v_lo + D_KV].set(DEEPNORM_BETA)
    w_in = nrm(ks[4], (L, D, D_IN), f32) * (D ** -0.5) * col_scale
    b_gate = 0.02 * nrm(ks[5], (L, 2 * D), f32)
    q_norm_g = 1.0 + 0.02 * nrm(ks[6], (L, HEAD_DIM), f32)
    k_norm_g = 1.0 + 0.02 * nrm(ks[7], (L, HEAD_DIM), f32)
    w_attn_o = nrm(ks[8], (L, D_ATTN, D), f32) * (D_ATTN ** -0.5) * DEEPNORM_BETA
    conv_w = nrm(ks[9], (L, CONV_WIDTH, D_CONV), f32) * (CONV_WIDTH ** -0.5)
    w_conv_o = nrm(ks[10], (L, D_CONV, D), f32) * (D_CONV ** -0.5) * DEEPNORM_BETA
    w_out = nrm(ks[11], (L, D, D), f32) * (D ** -0.5) * DEEPNORM_BETA
    ln1_g = 1.0 + 0.02 * nrm(ks[12], (L, D), f32)
    ln1_b = 0.02 * nrm(ks[13], (L, D), f32)
    w_group_router = nrm(ks[14], (L, D, G), f32) * (D ** -0.5)
    b_group_router = 0.01 * nrm(ks[15], (L, G), f32)
    w_expert_router = nrm(ks[16], (L, D, G, E), f32) * (D ** -0.5)
    b_expert_router = 0.01 * nrm(ks[17], (L, G, E), f32)
    w_exp_gate = nrm(ks[18], (L, G, E, D, F), f32) * (D ** -0.5)
    w_exp_up = nrm(ks[19], (L, G, E, D, F), f32) * (D ** -0.5)
    w_exp_down = nrm(ks[20], (L, G, E, F, D), f32) * (F ** -0.5) * DEEPNORM_BETA
    w_ple = nrm(ks[21], (L, PLE_DIM, D), f32) * (PLE_DIM ** -0.5) * DEEPNORM_BETA
    w_ple_gate = nrm(ks[22], (L, D, D), f32) * (D ** -0.5)
    b_ple_gate = 0.02 * nrm(ks[23], (L, D), f32)
    ln2_g = 1.0 + 0.02 * nrm(ks[24], (L, D), f32)
    ln2_b = 0.02 * nrm(ks[25], (L, D), f32)
    return {"x": x, "p": p, "emb_ln_g": emb_ln_g, "emb_ln_b": emb_ln_b,
            "w_in": w_in, "b_gate": b_gate, "q_norm_g": q_norm_g, "k_norm_g": k_norm_g,
            "w_attn_o": w_attn_o, "conv_w": conv_w, "w_conv_o": w_conv_o, "w_out": w_out,
            "ln1_g": ln1_g, "ln1_b": ln1_b,
            "w_group_router": w_group_router, "b_group_router": b_group_router,
            "w_expert_router": w_expert_router, "b_expert_router": b_expert_router,
            "w_exp_gate": w_exp_gate, "w_exp_up": w_exp_up, "w_exp_down": w_exp_down,
            "w_ple": w_ple, "w_ple_gate": w_ple_gate, "b_ple_gate": b_ple_gate,
            "ln2_g": ln2_g, "ln2_b": ln2_b}


def reference(x, p, emb_ln_g, emb_ln_b, w_in, b_gate, q_norm_g, k_norm_g, w_attn_o, conv_w,
              w_conv_o, w_out, ln1_g, ln1_b, w_group_router, b_group_router, w_expert_router,
              b_expert_router, w_exp_gate, w_exp_up, w_exp_down, w_ple, w_ple_gate, b_ple_gate,
              ln2_g, ln2_b):
    b, s, d = x.shape
    ang_r, ang_c = axial_angles(s)
    splits = _split_points()
    x = layer_norm(x, emb_ln_g, emb_ln_b)
    for i in range(DEPTH):
        proj = jnp.matmul(x, w_in[i])
        q, k, v, cb, cc, ch, ga, gc = jnp.split(proj, splits, axis=-1)
        q = axial_rope(head_rms_norm(q.reshape(b, s, N_HEADS, HEAD_DIM), q_norm_g[i]), ang_r, ang_c)
        k = axial_rope(head_rms_norm(k.reshape(b, s, N_KV_HEADS, HEAD_DIM), k_norm_g[i]), ang_r, ang_c)
        v = v.reshape(b, s, N_KV_HEADS, HEAD_DIM)
        attn = jnp.matmul(bidir_gqa(q, k, v), w_attn_o[i])
        conv = jnp.matmul(cb * short_conv_centred(cc * ch, conv_w[i]), w_conv_o[i])
        g_a = jax.nn.sigmoid(ga + b_gate[i, :d])
        g_c = jax.nn.sigmoid(gc + b_gate[i, d:])
        mix = jnp.matmul(g_a * attn + g_c * conv, w_out[i])
        x = layer_norm(DEEPNORM_ALPHA * x + mix, ln1_g[i], ln1_b[i])
        xt = x.reshape(b * s, d)
        moe = hierarchical_moe(xt, w_group_router[i], b_group_router[i], w_expert_router[i],
                               b_expert_router[i], w_exp_gate[i], w_exp_up[i], w_exp_down[i]).reshape(b, s, d)
        ple = jax.nn.sigmoid(jnp.matmul(x, w_ple_gate[i]) + b_ple_gate[i]) * jnp.matmul(p[i], w_ple[i])
        x = layer_norm(DEEPNORM_ALPHA * x + moe + ple, ln2_g[i], ln2_b[i])
    return x
```

```python
import os
from contextlib import ExitStack
import numpy as np
import concourse.bass as bass
import concourse.mybir as mybir
from concourse.bass_utils import run_bass_kernel_spmd

F32 = mybir.dt.float32
BF16 = mybir.dt.bfloat16
AF = mybir.ActivationFunctionType
ALU = mybir.AluOpType

NCORES = 8
S = 8192
D = 2048
TOK = 1024
TOKX = 1026
ALPHA = float(2.0 ** 0.25)
LN_EPS = 1e-5
QK_EPS = 1e-6
O_Q, O_K, O_V, O_CB, O_CC, O_CH, O_GA, O_GC = 0, 2048, 2560, 3072, 5120, 7168, 9216, 11264
SB_BASE = 16512
SB_END = 229376
NSLOT = 4
SLOT_BYTES = 8192
SCRATCH_KIND = "Internal"


class Buf:
    __slots__ = ("name", "lw", "rd", "excl")

    def __init__(self, name="", excl=False):
        self.name = name
        self.lw = None
        self.rd = {}
        self.excl = excl


class DmaSem:
    __slots__ = ("handle", "total", "name")

    def __init__(self, handle, name):
        self.handle = handle
        self.total = 0
        self.name = name


class Ins:
    __slots__ = ("eng", "fn", "deps", "sig", "sigval", "is_dma", "dsem", "dval", "ndma")

    def __init__(self, eng, fn):
        self.eng = eng
        self.fn = fn
        self.deps = {}
        self.sig = False
        self.sigval = 0
        self.is_dma = False
        self.dsem = None
        self.dval = 0
        self.ndma = 0

    def key(self):
        return ("d", id(self.dsem)) if self.is_dma else ("e", self.eng)


ENGS = ("pe", "act", "dve", "pool", "sp")


class Prog:
    def __init__(self, nc, stack):
        self.nc = nc
        self.stack = stack
        self.streams = {e: [] for e in ENGS}
        self.pending = {e: {} for e in ENGS}
        self.esem = {}
        self.dsems = []
        self.all_dma = {}
        for e in ENGS:
            if e != "sp":
                self.esem[e] = stack.enter_context(nc.semaphore("es_" + e))

    def dsem(self, name):
        h = self.stack.enter_context(self.nc.semaphore("ds_" + name))
        d = DmaSem(h, name)
        self.dsems.append(d)
        return d

    @staticmethod
    def _later(a, b):
        if a.is_dma:
            return a.dval > b.dval
        return a.sigval > b.sigval

    def _add(self, ins, reads, writes):
        deps = ins.deps
        if any(b.excl for b in reads):
            writes = list(writes) + [b for b in reads if b.excl]
            reads = [b for b in reads if not b.excl]

        def add(d):
            if d is None:
                return
            if (not d.is_dma) and (not ins.is_dma) and d.eng == "pe" and ins.eng == "pe":
                return
            k = d.key()
            o = deps.get(k)
            if o is None or self._later(d, o):
                deps[k] = d

        for b in reads:
            add(b.lw)
        for b in writes:
            add(b.lw)
            for d in b.rd.values():
                add(d)
        for d in self.pending[ins.eng].values():
            add(d)
        self.pending[ins.eng] = {}
        for d in deps.values():
            d.sig = True
        for b in reads:
            b.rd[ins.key()] = ins
        for b in writes:
            b.lw = ins
            b.rd = {}
        self.streams[ins.eng].append(ins)
        ins.sigval = len(self.streams[ins.eng])
        return ins

    def op(self, eng, fn, reads=(), writes=()):
        return self._add(Ins(eng, fn), reads, writes)

    def dma(self, eng, fn, ndma, sem, reads=(), writes=()):
        ins = Ins(eng, fn)
        ins.is_dma = True
        ins.dsem = sem
        ins.ndma = ndma
        sem.total += 16 * ndma
        ins.dval = sem.total
        self.all_dma[id(sem)] = ins
        return self._add(ins, reads, writes)

    def barrier(self):
        last = {}
        for e in ENGS:
            for i in reversed(self.streams[e]):
                if not i.is_dma:
                    last[("e", e)] = i
                    break
        for k, d in self.all_dma.items():
            last[("d", k)] = d
        for e in ENGS:
            self.pending[e] = dict(last)

    def emit(self):
        nc = self.nc
        for e in ENGS:
            c = 0
            for i in self.streams[e]:
                if i.is_dma:
                    continue
                if i.sig:
                    c += 1
                    i.sigval = c
                else:
                    i.sigval = -1
        streams = self.streams
        esem = self.esem

        def run(e, eng):
            waited = {}
            for ins in streams[e]:
                for k, d in ins.deps.items():
                    if d.is_dma:
                        sem, val = d.dsem.handle, d.dval
                    else:
                        assert d.sigval > 0
                        sem, val = esem[d.eng], d.sigval
                    if waited.get(k, 0) >= val:
                        continue
                    waited[k] = val
                    eng.wait_ge(sem, val)
                r = ins.fn(eng)
                if ins.is_dma:
                    assert len(r) == ins.ndma, (len(r), ins.ndma)
                    for x in r:
                        x.then_inc(ins.dsem.handle, 16)
                elif ins.sig:
                    r.then_inc(esem[e], 1)

        with nc.Block() as block:
            @block.tensor
            def _(eng):
                run("pe", eng)

            @block.scalar
            def _(eng):
                run("act", eng)

            @block.vector
            def _(eng):
                run("dve", eng)

            @block.gpsimd
            def _(eng):
                run("pool", eng)

            @block.sync
            def _(eng):
                run("sp", eng)
                for d in self.dsems:
                    if d.total > 0:
                        eng.wait_ge(d.handle, d.total)


class Arena:
    def __init__(self, nc, lo, hi, tag):
        self.nc, self.lo, self.hi, self.off, self.tag, self.n = nc, lo, hi, lo, tag, 0

    def reset(self, lo=None, hi=None):
        if lo is not None:
            self.lo = lo
        if hi is not None:
            self.hi = hi
        self.off = self.lo

    def alloc(self, shape, dt):
        sz = int(np.prod(shape[1:])) * mybir.dt.size(dt)
        sz = (sz + 63) // 64 * 64
        assert self.off + sz <= self.hi, ("SBUF arena overflow", self.tag, self.off, sz, self.hi)
        self.n += 1
        t = self.nc.alloc_sbuf_tensor_at("%s_%d" % (self.tag, self.n), list(shape), dt, offset=self.off)
        self.off += sz
        return t


def build(dbg=None, stop=None):
    nc = bass.Bass("TRN2", target_bir_lowering=False)

    def din(name, shape, dt=F32):
        return nc.dram_tensor(name, list(shape), dt, kind="ExternalInput").ap()

    x_all = din("x_all", [S, D])
    x_own = din("x_own", [TOK, D])
    x_halo = din("x_halo", [2, D])
    pT = din("pT", [256, TOK])
    cos_all = din("cos_all", [128, S])
    sin_all = din("sin_all", [128, S])
    cos_own = din("cos_own", [128, TOK])
    sin_own = din("sin_own", [128, TOK])
    w_in = din("w_in", [D, 13312])
    w_ao = din("w_ao", [D, D])
    w_co = din("w_co", [D, D])
    w_out = din("w_out", [D, D])
    w_pg = din("w_pg", [D, D])
    w_ple = din("w_ple", [256, D])
    w1 = din("w1", [16, D, 1024])
    w3 = din("w3", [16, D, 1024])
    w2 = din("w2", [16, 1024, D])
    vecs_d = din("vecs", [128, 12, 16])
    cmat_d = din("cmat", [128, 2, 128])
    gqk_d = din("gqk", [128, 2])
    gqk_row_d = din("gqk_row", [128, 2, 128])
    wr_d = din("wr", [128, 16, 20])
    br_d = din("br", [128, 20])
    sel_d = din("sel", [16, 16, 128])
    hmask_d = din("hmask", [128, 2])
    out = nc.dram_tensor("out", [TOK, D], F32, kind="ExternalOutput").ap()
    kT_s = nc.dram_tensor("kT_s", [4, 128, S], BF16, kind=SCRATCH_KIND).ap()
    v_s = nc.dram_tensor("v_s", [S, 512], BF16, kind=SCRATCH_KIND).ap()
    xres_s = nc.dram_tensor("xres_s", [128, 16, TOK], F32, kind=SCRATCH_KIND).ap()
    dbg_out = {}
    if dbg:
        for name, shape, dt in dbg:
            dbg_out[name] = nc.dram_tensor("dbg_" + name, list(shape), dt, kind="ExternalOutput").ap()

    st = ExitStack()
    with st:
        p = Prog(nc, st)

        def OP(eng, meth, reads=(), writes=(), **kw):
            p.op(eng, lambda e: getattr(e, meth)(**kw), reads, writes)

        def DMA(eng, sem, pairs, reads=(), writes=()):
            pairs = list(pairs)
            p.dma(eng, lambda e: [e.dma_start(out=o, in_=i) for o, i in pairs], len(pairs), sem, reads, writes)

        def MM(bank, lhsT, rhs, start, stop_, reads, cols=None):
            o = ps[bank][:] if cols is None else ps[bank][:, cols[0]:cols[1]]
            OP("pe", "matmul", reads=reads, writes=[psB[bank]], out=o, lhsT=lhsT, rhs=rhs, start=start, stop=stop_)

        def finish():
            p.emit()
            return nc

        def dbg_dump(name, src_ap, reads):
            if name in dbg_out:
                DMA("sp", p.dsem("dbg_" + name), [(dbg_out[name], src_ap)], reads=reads)

        off = SB_BASE
        misc = Arena(nc, off, off + 3584, "misc"); off += 3584
        slots_lo = off
        off += NSLOT * SLOT_BYTES
        q_lo = off
        off += 16384
        r2_lo = off
        off += 32896
        r3_lo = off; off += 32768
        r4_lo = off; off += 32768
        r5_lo = off; off += 32768
        r6_lo = off
        tmp = Arena(nc, r6_lo, SB_END, "tmp")
        aux = Arena(nc, r2_lo, r3_lo, "aux")

        ident_f = misc.alloc([128, 128], F32)
        rmat_f = misc.alloc([128, 128], F32)
        ident_b = misc.alloc([128, 128], BF16)
        ones_b = misc.alloc([128, 128], BF16)
        ones_f = misc.alloc([128, 128], F32)
        vecs = misc.alloc([128, 12, 16], F32)
        gqk = misc.alloc([128, 2], F32)
        negc = misc.alloc([128, 1], F32)
        eps_ln = misc.alloc([128, 1], F32)
        eps_qk = misc.alloc([128, 1], F32)
        hmask = misc.alloc([128, 2], F32)
        V_EG, V_EB, V_L1G, V_L1B, V_L2G, V_L2B, V_BGA, V_BGC, V_BPG, V_CW0, V_CW1, V_CW2 = range(12)
        B_const = Buf("const")
        xT_own = nc.alloc_sbuf_tensor_at("xT_own", [128, 16, TOKX], BF16, offset=r2_lo)
        B_xT = Buf("xT_own")
        sT = nc.alloc_sbuf_tensor_at("sT", [128, 16, TOK], BF16, offset=r3_lo)
        x1T = sT
        B_r3 = Buf("r3")
        gqaT = nc.alloc_sbuf_tensor_at("gqaT", [128, 16, TOK], BF16, offset=r4_lo)
        convinT = gqaT
        B_r4 = Buf("r4")
        kT_g = nc.alloc_sbuf_tensor_at("kT_g", [128, S], BF16, offset=r5_lo)
        v_g = nc.alloc_sbuf_tensor_at("v_g", [128, 64, 128], BF16, offset=r5_lo + 16384)
        B_kv = Buf("kv")
        accT = nc.alloc_sbuf_tensor_at("accT", [128, 16, TOK], F32, offset=r4_lo)
        B_acc = [Buf("acc%d" % c) for c in range(16)]
        slots = [nc.alloc_sbuf_tensor_at("slot%d" % i, [128, SLOT_BYTES // 2], BF16, offset=slots_lo + i * SLOT_BYTES)
                 for i in range(NSLOT)]
        B_slot = [Buf("slot%d" % i) for i in range(NSLOT)]

        ps = [nc.alloc_psum_tensor("ps%d" % i, [128, 512], F32) for i in range(8)]
        psB = [Buf("ps%d" % i, excl=True) for i in range(8)]

        d_c = p.dsem("const")
        d_x = [p.dsem("x%d" % i) for i in range(4)]
        d_o = p.dsem("store")
        d_t = [p.dsem("tbl%d" % i) for i in range(2)]
        d_kv = p.dsem("kv")
        d_slot = [p.dsem("slot%d" % i) for i in range(NSLOT)]

        def vcol(i, c):
            return vecs[:, i, c:c + 1]

        DMA("sp", d_c, [(ident_f[:], cmat_d[:, 0, :]), (rmat_f[:], cmat_d[:, 1, :]), (vecs[:], vecs_d),
                        (gqk[:], gqk_d), (hmask[:], hmask_d)], writes=[B_const])
        OP("dve", "tensor_copy", reads=[B_const], writes=[B_const], out=ident_b[:], in_=ident_f[:])
        OP("dve", "memset", writes=[B_const], ap=ones_b[:], constant=1.0 / 128.0)
        OP("dve", "memset", writes=[B_const], ap=ones_f[:], constant=1.0 / 2048.0)
        OP("dve", "memset", writes=[B_const], ap=eps_ln[:], constant=LN_EPS)
        OP("dve", "memset", writes=[B_const], ap=eps_qk[:], constant=QK_EPS)
        tmp.reset(r3_lo, SB_END)
        grow = tmp.alloc([128, 2, 128], F32)
        gmx = tmp.alloc([128, 2], F32)
        B_g = Buf("grow")
        DMA("sp", p.dsem("grow"), [(grow[:], gqk_row_d)], writes=[B_g])
        OP("dve", "tensor_reduce", reads=[B_g], writes=[B_g], out=gmx[:], in_=grow[:], axis=mybir.AxisListType.X,
           op=ALU.max, apply_absolute_value=True)
        OP("dve", "scalar_tensor_tensor", reads=[B_g], writes=[B_const], out=negc[:], in0=gmx[:, 0:1],
           scalar=-float(np.sqrt(128.0)), in1=gmx[:, 1:2], op0=ALU.mult, op1=ALU.mult)
        p.barrier()

        def ln_tile(xin_t, B_x, z_t, B_z, T, npart=128):
            st6, mv, rs, nm, B_t = T
            for q in range(4):
                OP("dve", "bn_stats", reads=[B_x], writes=[B_t], out=st6[0:npart, q, :],
                   in_=xin_t[0:npart, q * 512:(q + 1) * 512])
            OP("dve", "bn_aggr", reads=[B_t], writes=[B_t], out=mv[0:npart, :], in_=st6[0:npart, :, :])
            OP("act", "activation", reads=[B_t, B_const], writes=[B_t], out=rs[0:npart, :], in_=mv[0:npart, 1:2],
               func=AF.Sqrt, bias=eps_ln[0:npart, :], scale=1.0)
            OP("dve", "reciprocal", reads=[B_t], writes=[B_t], out=rs[0:npart, :], in_=rs[0:npart, :])
            OP("dve", "scalar_tensor_tensor", reads=[B_t], writes=[B_t], out=nm[0:npart, :], in0=mv[0:npart, 0:1],
               scalar=-1.0, in1=rs[0:npart, :], op0=ALU.mult, op1=ALU.mult)
            OP("act", "activation", reads=[B_x, B_t], writes=[B_z], out=z_t[0:npart, :], in_=xin_t[0:npart, :],
               func=AF.Identity, bias=nm[0:npart, :], scale=rs[0:npart, :])

        def ln_temps(n):
            return [(tmp.alloc([128, 4, 6], F32), tmp.alloc([128, 2], F32), tmp.alloc([128, 1], F32),
                     tmp.alloc([128, 1], F32), Buf("lnt%d" % i)) for i in range(n)]

        def rms_rope(bank, gcol, cos_ap, sin_ap, B_tab, out_ap, B_out, T, psx0, psx1):
            sq, rt, kn, B_sq, B_rt, B_kn = T
            src = ps[bank][:]
            OP("act", "activation", reads=[psB[bank]], writes=[B_sq], out=sq[:], in_=src, func=AF.Square)
            MM(psx0, ones_b[:], sq[:], True, True, [B_sq, B_const])
            OP("act", "activation", reads=[psB[psx0], B_const], writes=[B_rt], out=rt[:], in_=ps[psx0][:], func=AF.Sqrt,
               bias=eps_qk[:], scale=1.0)
            OP("dve", "reciprocal", reads=[B_rt], writes=[B_rt], out=rt[:], in_=rt[:])
            OP("dve", "scalar_tensor_tensor", reads=[psB[bank], B_rt, B_const], writes=[B_kn], out=kn[:], in0=src,
               scalar=gcol, in1=rt[:], op0=ALU.mult, op1=ALU.mult)
            MM(psx1, rmat_f[:], kn[:], True, True, [B_kn, B_const])
            OP("dve", "tensor_tensor", reads=[psB[psx1], B_tab], writes=[B_rt], out=rt[:], in0=ps[psx1][:], in1=sin_ap,
               op=ALU.mult)
            OP("pool", "tensor_tensor", reads=[B_kn, B_tab], writes=[B_kn], out=kn[:], in0=kn[:], in1=cos_ap, op=ALU.mult)
            OP("pool", "tensor_tensor", reads=[B_kn, B_rt], writes=[B_out], out=out_ap, in0=kn[:], in1=rt[:], op=ALU.add)

        def chain_temps(n):
            return [(tmp.alloc([128, 512], BF16), tmp.alloc([128, 512], F32), tmp.alloc([128, 512], F32),
                     Buf("sq%d" % i), Buf("rt%d" % i), Buf("kn%d" % i)) for i in range(n)]

        tmp.reset(r3_lo, SB_END)
        xin = [tmp.alloc([128, D], F32) for _ in range(4)]
        B_xin = [Buf("xin%d" % i) for i in range(4)]
        zf = [tmp.alloc([128, D], F32) for _ in range(4)]
        B_zf = [Buf("zf%d" % i) for i in range(4)]
        stage = tmp.alloc([128, 16, 512], F32)
        B_stage = Buf("stage")
        lnT = ln_temps(4)
        for t in range(2):
            for s4 in range(4):
                sub = t * 4 + s4
                DMA("sp", d_x[s4], [(xin[s4][:], x_own[sub * 128:(sub + 1) * 128, :])], writes=[B_xin[s4]])
                ln_tile(xin[s4], B_xin[s4], zf[s4], B_zf[s4], lnT[s4])
            for c in range(16):
                pb = c % 2
                for s4 in range(4):
                    OP("pe", "transpose", reads=[B_zf[s4], B_const], writes=[psB[pb]],
                       out=ps[pb][:, s4 * 128:(s4 + 1) * 128], in_=zf[s4][:, c * 128:(c + 1) * 128], identity=ident_f[:])
                OP("dve", "tensor_scalar", reads=[psB[pb], B_const], writes=[B_stage], out=stage[:, c, :], in0=ps[pb][:],
                   scalar1=vcol(V_EG, c), scalar2=vcol(V_EB, c), op0=ALU.mult, op1=ALU.add)
                OP("act", "activation", reads=[B_stage], writes=[B_xT], out=xT_own[:, c, t * 512:(t + 1) * 512],
                   in_=stage[:, c, :], func=AF.Copy)
            DMA("sp", d_o, [(xres_s[:, :, t * 512:(t + 1) * 512], stage[:])], reads=[B_stage])
        DMA("sp", d_x[0], [(xin[0][0:2, :], x_halo)], writes=[B_xin[0]])
        ln_tile(xin[0], B_xin[0], zf[0], B_zf[0], lnT[0], npart=2)
        for c in range(16):
            OP("pe", "transpose", reads=[B_zf[0], B_const], writes=[psB[2]], out=ps[2][:, c * 2:(c + 1) * 2],
               in_=zf[0][0:2, c * 128:(c + 1) * 128], identity=ident_f[0:2, 0:2])
        htmp = tmp.alloc([128, 16, 2], F32)
        B_ht = Buf("htmp")
        OP("dve", "tensor_tensor", reads=[psB[2], B_const], writes=[B_ht], out=htmp[:],
           in0=ps[2][:, 0:32].rearrange("p (c t) -> p c t", t=2),
           in1=vecs[:, V_EG, :].unsqueeze(2).broadcast_to([128, 16, 2]), op=ALU.mult)
        OP("dve", "tensor_tensor", reads=[B_ht, B_const], writes=[B_xT], out=xT_own[:, :, TOK:TOKX], in0=htmp[:],
           in1=vecs[:, V_EB, :].unsqueeze(2).broadcast_to([128, 16, 2]), op=ALU.add)
        dbg_dump("xT_own", xT_own[:], [B_xT])
        p.barrier()
        if stop == "B":
            return finish()

        tmp.reset(slots_lo, r2_lo)
        wkv = tmp.alloc([128, 16, 512], BF16)
        wkv2 = tmp.alloc([128, 16, 512], BF16)
        B_wkv = Buf("wkv")
        tmp.reset(r3_lo, SB_END)
        xTt = [tmp.alloc([128, 16, 512], BF16) for _ in range(2)]
        B_xTt = [Buf("xTt0"), Buf("xTt1")]
        xinA = [tmp.alloc([128, D], F32) for _ in range(4)]
        zb = [tmp.alloc([128, D], BF16) for _ in range(8)]
        B_zb = [Buf("zb%d" % i) for i in range(8)]
        rope = [(tmp.alloc([128, 512], F32), tmp.alloc([128, 512], F32)) for _ in range(2)]
        B_rope = [Buf("rope0"), Buf("rope1")]
        cT = chain_temps(2)
        kout = [tmp.alloc([128, 512], BF16) for _ in range(2)]
        B_kout = [Buf("kout0"), Buf("kout1")]
        vout = [tmp.alloc([128, 512], BF16) for _ in range(2)]
        B_vout = [Buf("vout0"), Buf("vout1")]
        lnTA = ln_temps(4)
        d_ko = [p.dsem("kout0"), p.dsem("kout1")]
        d_vo = [p.dsem("vout0"), p.dsem("vout1")]
        w_in_v = w_in.rearrange("(c p) n -> p c n", p=128)
        DMA("pool", p.dsem("wkv"), [(wkv[:], w_in_v[:, :, O_K:O_K + 512]), (wkv2[:], w_in_v[:, :, O_V:O_V + 512])],
            writes=[B_wkv])
        NT = 1 if stop == "A1" else 16

        def A_ldma_x(t):
            for s4 in range(4):
                sub = t * 4 + s4
                DMA("sp", d_x[s4], [(xinA[s4][:], x_all[sub * 128:(sub + 1) * 128, :])], writes=[B_xin[s4]])

        def A_ldma_rope(t):
            tp = t % 2
            DMA("sp", d_t[tp], [(rope[tp][0][:], cos_all[:, t * 512:(t + 1) * 512]),
                                (rope[tp][1][:], sin_all[:, t * 512:(t + 1) * 512])], writes=[B_rope[tp]])

        def A_ln(t, s4):
            zi = (t % 2) * 4 + s4
            ln_tile(xinA[s4], B_xin[s4], zb[zi], B_zb[zi], lnTA[s4])

        def A_T(t, c0, c1):
            tp = t % 2
            for c in range(c0, c1):
                pb = c % 2
                pv = ps[pb][:].bitcast(BF16)
                for s4 in range(4):
                    zi = tp * 4 + s4
                    OP("pe", "transpose", reads=[B_zb[zi], B_const], writes=[psB[pb]], out=pv[:, s4 * 128:(s4 + 1) * 128],
                       in_=zb[zi][:, c * 128:(c + 1) * 128], identity=ident_b[:])
                OP("act", "activation", reads=[psB[pb], B_const], writes=[B_xTt[tp]], out=xTt[tp][:, c, :],
                   in_=pv[:, 0:512], func=AF.Identity, bias=vcol(V_EB, c), scale=vcol(V_EG, c))

        def A_fin(t, h):
            hb, tp = h % 2, t % 2
            sq, rt, kn, B_sq, B_rt, B_kn = cT[hb]
            MM(5, rmat_f[:], kn[:], True, True, [B_kn, B_const])
            OP("dve", "tensor_tensor", reads=[psB[5], B_rope[tp]], writes=[B_rt], out=rt[:], in0=ps[5][:],
               in1=rope[tp][1][:], op=ALU.mult)
            OP("pool", "tensor_tensor", reads=[B_kn, B_rope[tp]], writes=[B_kn], out=kn[:], in0=kn[:], in1=rope[tp][0][:],
               op=ALU.mult)
            OP("pool", "tensor_tensor", reads=[B_kn, B_rt], writes=[B_kout[hb]], out=kout[hb][:], in0=kn[:], in1=rt[:],
               op=ALU.add)
            DMA("pool", d_ko[hb], [(kT_s[h, :, t * 512:(t + 1) * 512], kout[hb][:])], reads=[B_kout[hb]])

        def A_KV(t, h):
            hb, tp = h % 2, t % 2
            sq, rt, kn, B_sq, B_rt, B_kn = cT[hb]
            for c in range(16):
                MM(2 + hb, wkv[:, c, h * 128:(h + 1) * 128], xTt[tp][:, c, :], c == 0, c == 15, [B_wkv, B_xTt[tp]])
            OP("act", "activation", reads=[psB[2 + hb]], writes=[B_sq], out=sq[:], in_=ps[2 + hb][:], func=AF.Square)
            for c in range(16):
                MM(6 + hb, xTt[tp][:, c, h * 128:(h + 1) * 128], wkv2[:, c, :], c == 0, c == 15, [B_wkv, B_xTt[tp]])
            MM(4, ones_b[:], sq[:], True, True, [B_sq, B_const])
            OP("act", "activation", reads=[psB[6 + hb]], writes=[B_vout[hb]], out=vout[hb][:], in_=ps[6 + hb][:], func=AF.Copy)
            r0 = (t * 4 + h) * 128
            DMA("pool", d_vo[hb], [(v_s[r0:r0 + 128, :], vout[hb][:])], reads=[B_vout[hb]])
            OP("act", "activation", reads=[psB[4], B_const], writes=[B_rt], out=rt[:], in_=ps[4][:], func=AF.Sqrt,
               bias=eps_qk[:], scale=1.0)
            if h > 0:
                A_fin(t, h - 1)
            OP("dve", "reciprocal", reads=[B_rt], writes=[B_rt], out=rt[:], in_=rt[:])
            OP("dve", "scalar_tensor_tensor", reads=[psB[2 + hb], B_rt, B_const], writes=[B_kn], out=kn[:], in0=ps[2 + hb][:],
               scalar=gqk[:, 1:2], in1=rt[:], op0=ALU.mult, op1=ALU.mult)

        for i in range(-2, NT):
            if 0 <= i + 1 < NT:
                A_ldma_rope(i + 1)
            if 0 <= i + 2 < NT:
                A_ldma_x(i + 2)
            for h in range(4):
                if 0 <= i < NT:
                    A_KV(i, h)
                if 0 <= i + 1 < NT:
                    A_T(i + 1, 4 * h, 4 * h + 4)
                if 0 <= i + 2 < NT:
                    A_ln(i + 2, h)
            if 0 <= i < NT:
                A_fin(i, 3)
        p.barrier()
        if "kT0" in dbg_out:
            DMA("sp", p.dsem("dbg2"), [(dbg_out["kT0"], kT_s[:, :, 0:512]), (dbg_out["v0"], v_s[0:512, :])])
        if stop in ("A", "A1"):
            return finish()

        def wcols(w, col0, n=256):
            return (w.rearrange("(c p) n -> p c n", p=128)[:, :, col0:col0 + n], 16, n)

        specs = []
        for g in range(4):
            for sl in range(2):
                specs.append(wcols(w_in, O_Q + g * 512 + sl * 256))
        for sg in range(8):
            specs.append(wcols(w_ao, sg * 256)); specs.append(wcols(w_in, O_GA + sg * 256))
        for sg in range(8):
            specs.append(wcols(w_in, O_CB + sg * 256)); specs.append(wcols(w_in, O_CC + sg * 256))
            specs.append(wcols(w_in, O_CH + sg * 256))
        for sg in range(8):
            specs.append(wcols(w_co, sg * 256)); specs.append(wcols(w_in, O_GC + sg * 256))
        for sg in range(8):
            specs.append(wcols(w_out, sg * 256))
        for sg in range(8):
            specs.append(wcols(w_pg, sg * 256))
        n_exp = int(os.environ.get("K_NEXP", "16"))
        for e in range(n_exp):
            for fq in range(4):
                specs.append(wcols(w1[e], fq * 256)); specs.append(wcols(w3[e], fq * 256))
            for cs4 in range(4):
                specs.append((w2[e].rearrange("(fc p) n -> p fc n", p=128)[:, :, cs4 * 512:(cs4 + 1) * 512], 8, 512))

        class WS:
            issued = 0
            nxt = 0

        def ws_issue():
            k = WS.issued
            src, a, b = specs[k]
            s_ = k % NSLOT
            dst = slots[s_][:, 0:a * b].rearrange("p (a b) -> p a b", a=a)
            DMA("pool", d_slot[s_], [(dst, src)], writes=[B_slot[s_]])
            WS.issued += 1

        def ws_next():
            k = WS.nxt
            WS.nxt += 1
            assert k < WS.issued, "weight stream underflow"
            src, a, b = specs[k]
            s_ = k % NSLOT
            return slots[s_][:, 0:a * b].rearrange("p (a b) -> p a b", a=a), B_slot[s_]

        def ws_done(n=1):
            for _ in range(n):
                if WS.issued < len(specs):
                    ws_issue()

        for _ in range(NSLOT):
            ws_issue()

        tmp.reset(r6_lo, SB_END)
        qT_b = [nc.alloc_sbuf_tensor_at("qT_g%d" % i, [128, 4, TOK], BF16, offset=q_lo + i * 8192) for i in range(2)]
        B_qb = [[Buf("q%d_%d" % (i, k)) for k in range(4)] for i in range(2)]
        tabq = [(tmp.alloc([128, 512], F32), tmp.alloc([128, 512], F32)) for _ in range(2)]
        B_tabq = Buf("tabq")
        cTq = chain_temps(1)
        qraw = tmp.alloc([128, 512], F32)
        B_qraw = Buf("qraw")
        NPT = 5
        pt = [tmp.alloc([128, 512], BF16) for _ in range(NPT)]
        B_pt = [Buf("pt%d" % i) for i in range(NPT)]
        NS2 = 3
        s2 = [tmp.alloc([128, 512], BF16) for _ in range(NS2)]
        B_s2 = [Buf("s2_%d" % i) for i in range(NS2)]
        rec = tmp.alloc([128, 512], F32)
        B_rec = Buf("rec")
        scale = float(128.0 ** -0.5)
        n_grp = int(os.environ.get("K_NGRP", "4"))
        DMA("sp", d_t[0], [(tabq[j][0][:], cos_own[:, j * 512:(j + 1) * 512]) for j in range(2)] +
            [(tabq[j][1][:], sin_own[:, j * 512:(j + 1) * 512]) for j in range(2)], writes=[B_tabq])
        wq_cur = [None, None]

        def q_stages(g, i):
            hl, j = i // 2, i % 2
            hh = hl % 2
            sq, rt, kn, B_sq, B_rt, B_kn = cTq[0]
            qdst = qT_b[g % 2][:, hl, j * 512:(j + 1) * 512]
            B_dst = B_qb[g % 2][hl]

            def st0():
                if i % 4 == 0:
                    wq_cur[0], wq_cur[1] = ws_next()
                wq, B_wq = wq_cur
                for c in range(16):
                    MM(7, wq[:, c, hh * 128:(hh + 1) * 128], xT_own[:, c, j * 512:(j + 1) * 512], c == 0, c == 15,
                       [B_wq, B_xT])
                if i % 4 == 3:
                    ws_done()
                OP("act", "activation", reads=[psB[7]], writes=[B_qraw], out=qraw[:], in_=ps[7][:], func=AF.Copy)
                OP("act", "activation", reads=[B_qraw], writes=[B_sq], out=sq[:], in_=qraw[:], func=AF.Square)

            def st1():
                MM(7, ones_b[:], sq[:], True, True, [B_sq, B_const])
                OP("act", "activation", reads=[psB[7], B_const], writes=[B_rt], out=rt[:], in_=ps[7][:], func=AF.Sqrt,
                   bias=eps_qk[:], scale=1.0)

            def st2():
                OP("dve", "reciprocal", reads=[B_rt], writes=[B_rt], out=rt[:], in_=rt[:])
                OP("dve", "scalar_tensor_tensor", reads=[B_qraw, B_rt, B_const], writes=[B_kn], out=kn[:], in0=qraw[:],
                   scalar=gqk[:, 0:1], in1=rt[:], op0=ALU.mult, op1=ALU.mult)

            def st3():
                MM(7, rmat_f[:], kn[:], True, True, [B_kn, B_const])

            def st4():
                OP("dve", "tensor_tensor", reads=[psB[7], B_tabq], writes=[B_rt], out=rt[:], in0=ps[7][:], in1=tabq[j][1][:],
                   op=ALU.mult)
                OP("pool", "tensor_tensor", reads=[B_kn, B_tabq], writes=[B_kn], out=kn[:], in0=kn[:], in1=tabq[j][0][:],
                   op=ALU.mult)

            def st5():
                OP("pool", "tensor_tensor", reads=[B_kn, B_rt], writes=[B_dst], out=qdst, in0=kn[:], in1=rt[:], op=ALU.add)

            return [st0, st1, st2, st3, st4, st5]

        for i in range(8):
            for f in q_stages(0, i):
                f()
        for g in range(n_grp):
            DMA("sp", d_kv, [(kT_g[:], kT_s[g]),
                             (v_g[:], v_s.rearrange("(kc p) (g d) -> p kc g d", p=128, g=4)[:, :, g, :])], writes=[B_kv])
            it = 0
            for hl in range(4):
                h = g * 4 + hl
                for j in range(2):
                    stages = q_stages(g + 1, it) if g + 1 < n_grp else []
                    po = 3 + it % 2
                    pd = 5 + it % 2
                    it += 1
                    qv = qT_b[g % 2][:, hl, j * 512:(j + 1) * 512]
                    B_qv = B_qb[g % 2][hl]

                    def qk(kc):
                        MM(kc % 3, kT_g[:, kc * 128:(kc + 1) * 128], qv, True, True, [B_kv, B_qv])

                    qk(0)
                    qk(1)
                    for kc in range(64):
                        b3 = kc % 3
                        pi = kc % NPT
                        OP("act", "activation", reads=[psB[b3], B_const], writes=[B_pt[pi]], out=pt[pi][:], in_=ps[b3][:],
                           func=AF.Exp, bias=negc[:], scale=scale)
                        if kc + 2 < 64:
                            qk(kc + 2)
                        MM(po, v_g[:, kc, :], pt[pi][:], kc == 0, kc == 63, [B_kv, B_pt[pi]])
                        if kc % 2 == 1:
                            pr = kc // 2
                            si = pr % NS2
                            pj = (kc - 1) % NPT
                            OP("dve", "tensor_tensor", reads=[B_pt[pj], B_pt[pi]], writes=[B_s2[si]], out=s2[si][:],
                               in0=pt[pj][:], in1=pt[pi][:], op=ALU.add)
                            if pr >= 1:
                                sj = (pr - 1) % NS2
                                MM(pd, ones_b[:], s2[sj][:], pr == 1, False, [B_const, B_s2[sj]])
                        if stages and kc % 8 == 4 and kc // 8 < len(stages):
                            stages[kc // 8]()
                    MM(pd, ones_b[:], s2[31 % NS2][:], False, True, [B_const, B_s2[31 % NS2]])
                    OP("dve", "reciprocal", reads=[psB[pd]], writes=[B_rec], out=rec[:], in_=ps[pd][:])
                    OP("dve", "scalar_tensor_tensor", reads=[psB[po], B_rec], writes=[B_r4], out=gqaT[:, h, j * 512:(j + 1) * 512],
                       in0=ps[po][:], scalar=1.0 / 128.0, in1=rec[:], op0=ALU.mult, op1=ALU.mult)
        dbg_dump("gqaT", gqaT[:], [B_r4])
        p.barrier()
        if stop == "D":
            return finish()

        tmp.reset(r6_lo, SB_END)
        sigt = [tmp.alloc([128, 512], F32) for _ in range(2)]
        B_sig = [Buf("sig0"), Buf("sig1")]
        it = 0
        for sg in range(8):
            sa, B_sa = ws_next()
            sgl, B_sgl = ws_next()
            for cc2 in range(2):
                c = sg * 2 + cc2
                cs = slice(cc2 * 128, (cc2 + 1) * 128)
                for j in range(2):
                    jr = slice(j * 512, (j + 1) * 512)
                    ba, bg, si = it % 2, 2 + it % 2, it % 2
                    it += 1
                    for k in range(16):
                        MM(ba, sa[:, k, cs], gqaT[:, k, jr], k == 0, k == 15, [B_sa, B_r4])
                    for k in range(16):
                        MM(bg, sgl[:, k, cs], xT_own[:, k, jr], k == 0, k == 15, [B_sgl, B_xT])
                    OP("act", "activation", reads=[psB[bg], B_const], writes=[B_sig[si]], out=sigt[si][:], in_=ps[bg][:],
                       func=AF.Sigmoid, bias=vcol(V_BGA, c), scale=1.0)
                    OP("dve", "tensor_tensor", reads=[psB[ba], B_sig[si]], writes=[B_r3], out=sT[:, c, jr], in0=ps[ba][:],
                       in1=sigt[si][:], op=ALU.mult)
            ws_done(2)
        p.barrier()

        tmp.reset(r6_lo, SB_END)
        ccs = tmp.alloc([128, TOK], F32)
        uext = tmp.alloc([128, TOKX], F32)
        c1 = tmp.alloc([128, TOK], F32)
        c2 = tmp.alloc([128, TOK], F32)
        hcp = tmp.alloc([128, 4], F32)
        uh = tmp.alloc([128, 2], F32)
        B_ccs, B_u, B_c1, B_c2, B_h = Buf("ccs"), Buf("uext"), Buf("c1"), Buf("c2"), Buf("hcp")
        hr = slice(TOK, TOKX)
        for sg in range(8):
            scb, B_scb = ws_next()
            scc, B_scc = ws_next()
            sch, B_sch = ws_next()
            for cc2 in range(2):
                c = sg * 2 + cc2
                cs = slice(cc2 * 128, (cc2 + 1) * 128)
                for j in range(2):
                    for k in range(16):
                        MM(j, scc[:, k, cs], xT_own[:, k, j * 512:(j + 1) * 512], k == 0, k == 15, [B_scc, B_xT])
                for k in range(16):
                    MM(6, scc[:, k, cs], xT_own[:, k, hr], k == 0, k == 15, [B_scc, B_xT], cols=(0, 2))
                for j in range(2):
                    for k in range(16):
                        MM(2 + j, sch[:, k, cs], xT_own[:, k, j * 512:(j + 1) * 512], k == 0, k == 15, [B_sch, B_xT])
                for k in range(16):
                    MM(6, sch[:, k, cs], xT_own[:, k, hr], k == 0, k == 15, [B_sch, B_xT], cols=(2, 4))
                for j in range(2):
                    for k in range(16):
                        MM(4 + j, scb[:, k, cs], xT_own[:, k, j * 512:(j + 1) * 512], k == 0, k == 15, [B_scb, B_xT])
                for j in range(2):
                    OP("act", "activation", reads=[psB[j]], writes=[B_ccs], out=ccs[:, j * 512:(j + 1) * 512], in_=ps[j][:],
                       func=AF.Copy)
                for j in range(2):
                    OP("dve", "tensor_tensor", reads=[psB[2 + j], B_ccs], writes=[B_u], out=uext[:, 1 + j * 512:1 + (j + 1) * 512],
                       in0=ps[2 + j][:], in1=ccs[:, j * 512:(j + 1) * 512], op=ALU.mult)
                OP("dve", "tensor_copy", reads=[psB[6]], writes=[B_h], out=hcp[:], in_=ps[6][:, 0:4])
                OP("dve", "tensor_tensor", reads=[B_h], writes=[B_h], out=uh[:], in0=hcp[:, 0:2], in1=hcp[:, 2:4], op=ALU.mult)
                OP("dve", "tensor_tensor", reads=[B_h, B_const], writes=[B_u], out=uext[:, 0:1], in0=uh[:, 0:1],
                   in1=hmask[:, 0:1], op=ALU.mult)
                OP("dve", "tensor_tensor", reads=[B_h, B_const], writes=[B_u], out=uext[:, TOK + 1:TOK + 2], in0=uh[:, 1:2],
                   in1=hmask[:, 1:2], op=ALU.mult)
                OP("act", "activation", reads=[B_u, B_const], writes=[B_c1], out=c1[:], in_=uext[:, 1:TOK + 1],
                   func=AF.Identity, scale=vcol(V_CW1, c))
                OP("dve", "scalar_tensor_tensor", reads=[B_u, B_c1, B_const], writes=[B_c2], out=c2[:], in0=uext[:, 0:TOK],
                   scalar=vcol(V_CW0, c), in1=c1[:], op0=ALU.mult, op1=ALU.add)
                OP("dve", "scalar_tensor_tensor", reads=[B_u, B_c2, B_const], writes=[B_c1], out=c1[:], in0=uext[:, 2:TOK + 2],
                   scalar=vcol(V_CW2, c), in1=c2[:], op0=ALU.mult, op1=ALU.add)
                for j in range(2):
                    OP("dve", "tensor_tensor", reads=[psB[4 + j], B_c1], writes=[B_r4], out=convinT[:, c, j * 512:(j + 1) * 512],
                       in0=ps[4 + j][:], in1=c1[:, j * 512:(j + 1) * 512], op=ALU.mult)
            ws_done(3)
        dbg_dump("convinT", convinT[:], [B_r4])
        p.barrier()
        if stop == "E1":
            return finish()

        tmp.reset(r6_lo, SB_END)
        sigt = [tmp.alloc([128, 512], F32) for _ in range(2)]
        tt = [tmp.alloc([128, 512], F32) for _ in range(2)]
        B_sig = [Buf("sig0"), Buf("sig1")]
        B_tt = [Buf("tt0"), Buf("tt1")]
        it = 0
        for sg in range(8):
            sco, B_sco = ws_next()
            sgc, B_sgc = ws_next()
            for cc2 in range(2):
                c = sg * 2 + cc2
                cs = slice(cc2 * 128, (cc2 + 1) * 128)
                for j in range(2):
                    jr = slice(j * 512, (j + 1) * 512)
                    ba, bg, si = it % 2, 2 + it % 2, it % 2
                    it += 1
                    for k in range(16):
                        MM(ba, sco[:, k, cs], convinT[:, k, jr], k == 0, k == 15, [B_sco, B_r4])
                    for k in range(16):
                        MM(bg, sgc[:, k, cs], xT_own[:, k, jr], k == 0, k == 15, [B_sgc, B_xT])
                    OP("act", "activation", reads=[psB[bg], B_const], writes=[B_sig[si]], out=sigt[si][:], in_=ps[bg][:],
                       func=AF.Sigmoid, bias=vcol(V_BGC, c), scale=1.0)
                    OP("dve", "tensor_tensor", reads=[psB[ba], B_sig[si]], writes=[B_tt[si]], out=tt[si][:], in0=ps[ba][:],
                       in1=sigt[si][:], op=ALU.mult)
                    OP("pool", "tensor_tensor", reads=[B_tt[si], B_r3], writes=[B_r3], out=sT[:, c, jr], in0=tt[si][:],
                       in1=sT[:, c, jr], op=ALU.add)
            ws_done(2)
        dbg_dump("sT", sT[:], [B_r3])
        p.barrier()

        tmp.reset(r6_lo, SB_END)
        xres = [tmp.alloc([128, TOK], F32) for _ in range(2)]
        B_xres = [Buf("xres0"), Buf("xres1")]
        d_xr = [p.dsem("xres0"), p.dsem("xres1")]
        it = 0
        for sg in range(8):
            so, B_so = ws_next()
            for cc2 in range(2):
                c = sg * 2 + cc2
                cs = slice(cc2 * 128, (cc2 + 1) * 128)
                xr = c % 2
                DMA("sp", d_xr[xr], [(xres[xr][:], xres_s[:, c, :])], writes=[B_xres[xr]])
                for j in range(2):
                    jr = slice(j * 512, (j + 1) * 512)
                    bm = it % 2
                    it += 1
                    for k in range(16):
                        MM(bm, so[:, k, cs], sT[:, k, jr], k == 0, k == 15, [B_so, B_r3])
                    OP("dve", "scalar_tensor_tensor", reads=[psB[bm], B_xres[xr]], writes=[B_acc[c]], out=accT[:, c, jr],
                       in0=xres[xr][:, jr], scalar=ALPHA, in1=ps[bm][:], op0=ALU.mult, op1=ALU.add)
            ws_done()
        p.barrier()

        def ln_feat(gi, bi, bf_out, B_bf):
            sqt = [tmp.alloc([128, 512], F32) for _ in range(2)]
            B_sqt = [Buf("sqt0"), Buf("sqt1")]
            mean_s = [tmp.alloc([128, 512], F32) for _ in range(2)]
            rstd = [tmp.alloc([128, 512], F32) for _ in range(2)]
            msq = tmp.alloc([128, 512], F32)
            tn = [tmp.alloc([128, 512], F32) for _ in range(2)]
            B_tn = [Buf("tn0"), Buf("tn1")]
            B_st = [Buf("lnstat0"), Buf("lnstat1")]
            B_msq = Buf("msq")
            for j in range(2):
                jr = slice(j * 512, (j + 1) * 512)
                for c in range(16):
                    MM(j, ones_f[:], accT[:, c, jr], c == 0, c == 15, [B_const, B_acc[c]])
                for c in range(16):
                    OP("act", "activation", reads=[B_acc[c]], writes=[B_sqt[c % 2]], out=sqt[c % 2][:], in_=accT[:, c, jr],
                       func=AF.Square)
                    MM(2 + j, ones_f[:], sqt[c % 2][:], c == 0, c == 15, [B_const, B_sqt[c % 2]])
            for j in range(2):
                OP("act", "activation", reads=[psB[j]], writes=[B_st[j]], out=mean_s[j][:], in_=ps[j][:], func=AF.Copy)
                OP("act", "activation", reads=[B_st[j]], writes=[B_msq], out=msq[:], in_=mean_s[j][:], func=AF.Square)
                OP("dve", "tensor_tensor", reads=[psB[2 + j], B_msq], writes=[B_st[j]], out=rstd[j][:], in0=ps[2 + j][:],
                   in1=msq[:], op=ALU.subtract)
                OP("act", "activation", reads=[B_st[j], B_const], writes=[B_st[j]], out=rstd[j][:], in_=rstd[j][:], func=AF.Sqrt,
                   bias=eps_ln[:], scale=1.0)
                OP("dve", "reciprocal", reads=[B_st[j]], writes=[B_st[j]], out=rstd[j][:], in_=rstd[j][:])
            for j in range(2):
                jr = slice(j * 512, (j + 1) * 512)
                for c in range(16):
                    ti = c % 2
                    OP("dve", "tensor_tensor", reads=[B_acc[c], B_st[j]], writes=[B_tn[ti]], out=tn[ti][:], in0=accT[:, c, jr],
                       in1=mean_s[j][:], op=ALU.subtract)
                    OP("pool" if ti == 0 else "dve", "tensor_tensor", reads=[B_tn[ti], B_st[j]], writes=[B_tn[ti]],
                       out=tn[ti][:], in0=tn[ti][:], in1=rstd[j][:], op=ALU.mult)
                    OP("act", "activation", reads=[B_tn[ti], B_const], writes=[B_acc[c]], out=accT[:, c, jr], in_=tn[ti][:],
                       func=AF.Identity, bias=vcol(bi, c), scale=vcol(gi, c))
                    if bf_out is not None:
                        OP("act", "activation", reads=[B_acc[c]], writes=[B_bf], out=bf_out[:, c, jr], in_=accT[:, c, jr],
                           func=AF.Copy)

        tmp.reset(r6_lo, SB_END)
        ln_feat(V_L1G, V_L1B, x1T, B_r3)
        dbg_dump("x1Tf", accT[:], B_acc)
        p.barrier()
        if stop == "E":
            return finish()

        tmp.reset(r6_lo, SB_END)
        sel = tmp.alloc([16, 16, 128], F32)
        combT = tmp.alloc([16, TOK], F32)
        B_sel, B_combT = Buf("sel"), Buf("combT")
        f_mark = tmp.off
        wr = tmp.alloc([128, 16, 20], F32)
        br = tmp.alloc([128, 20], F32)
        B_wr = Buf("wr")
        DMA("sp", p.dsem("wr"), [(wr[:], wr_d), (br[:], br_d), (sel[:], sel_d)], writes=[B_wr, B_sel])
        lg = tmp.alloc([128, 8, 20], F32)
        R = {k: tmp.alloc([128, 8, n], F32) for k, n in
             [("gmax", 1), ("gsh", 4), ("ge", 4), ("gsum", 1), ("gpt", 1), ("ohg", 4), ("els", 4), ("t4", 4), ("m1", 1),
              ("mask1", 4), ("els2", 4), ("m2", 1), ("mask2", 4), ("selm", 4), ("esh", 4), ("ee", 4), ("esum", 1),
              ("er", 1), ("ew", 4), ("gw", 4)]}
        comb = tmp.alloc([128, 8, 16], F32)
        B_rt_ = Buf("router")
        for s8 in range(8):
            for c in range(16):
                MM(0, accT[:, c, s8 * 128:(s8 + 1) * 128], wr[:, c, :], c == 0, c == 15, [B_acc[c], B_wr],
                   cols=(s8 * 20, (s8 + 1) * 20))

        def RO(meth, **kw):
            OP("dve", meth, reads=[B_rt_], writes=[B_rt_], **kw)

        def bc(ap, n):
            return ap.broadcast_to([128, 8, n])

        OP("dve", "tensor_tensor", reads=[psB[0], B_wr], writes=[B_rt_], out=lg[:],
           in0=ps[0][:, 0:160].rearrange("p (s n) -> p s n", n=20), in1=br[:].unsqueeze(1).broadcast_to([128, 8, 20]),
           op=ALU.add)
        gl = lg[:, :, 0:4]
        AXX = mybir.AxisListType.X
        RO("tensor_reduce", out=R["gmax"][:], in_=gl, axis=AXX, op=ALU.max)
        RO("tensor_tensor", out=R["gsh"][:], in0=gl, in1=bc(R["gmax"][:], 4), op=ALU.subtract)
        OP("act", "activation", reads=[B_rt_], writes=[B_rt_], out=R["ge"][:], in_=R["gsh"][:], func=AF.Exp)
        RO("tensor_reduce", out=R["gsum"][:], in_=R["ge"][:], axis=AXX, op=ALU.add)
        RO("reciprocal", out=R["gpt"][:], in_=R["gsum"][:])
        RO("tensor_tensor", out=R["ohg"][:], in0=gl, in1=bc(R["gmax"][:], 4), op=ALU.is_equal)
        for g in range(4):
            elg = lg[:, :, 4 + 4 * g:8 + 4 * g]
            if g == 0:
                RO("tensor_tensor", out=R["els"][:], in0=elg, in1=bc(R["ohg"][:, :, 0:1], 4), op=ALU.mult)
            else:
                RO("tensor_tensor", out=R["t4"][:], in0=elg, in1=bc(R["ohg"][:, :, g:g + 1], 4), op=ALU.mult)
                RO("tensor_tensor", out=R["els"][:], in0=R["els"][:], in1=R["t4"][:], op=ALU.add)
        RO("tensor_reduce", out=R["m1"][:], in_=R["els"][:], axis=AXX, op=ALU.max)
        RO("tensor_tensor", out=R["mask1"][:], in0=R["els"][:], in1=bc(R["m1"][:], 4), op=ALU.is_equal)
        RO("scalar_tensor_tensor", out=R["els2"][:], in0=R["mask1"][:], scalar=-1.0e30, in1=R["els"][:], op0=ALU.mult,
           op1=ALU.add)
        RO("tensor_reduce", out=R["m2"][:], in_=R["els2"][:], axis=AXX, op=ALU.max)
        RO("tensor_tensor", out=R["mask2"][:], in0=R["els2"][:], in1=bc(R["m2"][:], 4), op=ALU.is_equal)
        RO("tensor_tensor", out=R["selm"][:], in0=R["mask1"][:], in1=R["mask2"][:], op=ALU.add)
        RO("tensor_tensor", out=R["esh"][:], in0=R["els"][:], in1=bc(R["m1"][:], 4), op=ALU.subtract)
        OP("act", "activation", reads=[B_rt_], writes=[B_rt_], out=R["ee"][:], in_=R["esh"][:], func=AF.Exp)
        RO("tensor_tensor", out=R["ee"][:], in0=R["ee"][:], in1=R["selm"][:], op=ALU.mult)
        RO("tensor_reduce", out=R["esum"][:], in_=R["ee"][:], axis=AXX, op=ALU.add)
        RO("reciprocal", out=R["er"][:], in_=R["esum"][:])
        RO("tensor_tensor", out=R["ew"][:], in0=R["ee"][:], in1=bc(R["er"][:], 4), op=ALU.mult)
        RO("tensor_tensor", out=R["gw"][:], in0=R["ohg"][:], in1=bc(R["gpt"][:], 4), op=ALU.mult)
        RO("tensor_tensor", out=comb[:].rearrange("p s (g e) -> p s g e", g=4),
           in0=R["gw"][:].unsqueeze(3).broadcast_to([128, 8, 4, 4]),
           in1=R["ew"][:].unsqueeze(2).broadcast_to([128, 8, 4, 4]), op=ALU.mult)
        dbg_dump("comb", comb[:], [B_rt_])
        dbg_dump("logits", lg[:], [B_rt_])
        for s8 in range(8):
            bank = 1 + s8 // 4
            OP("pe", "transpose", reads=[B_rt_, B_const], writes=[psB[bank]],
               out=ps[bank][0:16, (s8 % 4) * 128:(s8 % 4 + 1) * 128], in_=comb[:, s8, :], identity=ident_f[:])
        for hf in range(2):
            OP("act", "activation", reads=[psB[1 + hf]], writes=[B_combT], out=combT[:, hf * 512:(hf + 1) * 512],
               in_=ps[1 + hf][0:16, :], func=AF.Copy)
        aux.reset()
        wple = aux.alloc([128, 2, D], BF16)
        pTs = aux.alloc([128, 2, TOK], BF16)
        B_ple = Buf("ple_in")
        DMA("pool", p.dsem("ple"), [(wple[:], w_ple.rearrange("(k p) n -> p k n", p=128)),
                                    (pTs[:], pT.rearrange("(k p) t -> p k t", p=128))], writes=[B_ple])
        sigt = [tmp.alloc([128, 512], F32) for _ in range(2)]
        tt = [tmp.alloc([128, 512], F32) for _ in range(2)]
        B_sig = [Buf("sig0"), Buf("sig1")]
        B_tt = [Buf("tt0"), Buf("tt1")]
        it = 0
        for sg in range(8):
            spg, B_spg = ws_next()
            for cc2 in range(2):
                c = sg * 2 + cc2
                cs = slice(cc2 * 128, (cc2 + 1) * 128)
                for j in range(2):
                    jr = slice(j * 512, (j + 1) * 512)
                    bg, bl, si = 3 + it % 2, 5 + it % 2, it % 2
                    it += 1
                    for k in range(16):
                        MM(bg, spg[:, k, cs], x1T[:, k, jr], k == 0, k == 15, [B_spg, B_r3])
                    for k in range(2):
                        MM(bl, wple[:, k, c * 128:(c + 1) * 128], pTs[:, k, jr], k == 0, k == 1, [B_ple])
                    OP("act", "activation", reads=[psB[bg], B_const], writes=[B_sig[si]], out=sigt[si][:], in_=ps[bg][:],
                       func=AF.Sigmoid, bias=vcol(V_BPG, c), scale=1.0)
                    OP("dve", "tensor_tensor", reads=[psB[bl], B_sig[si]], writes=[B_tt[si]], out=tt[si][:], in0=ps[bl][:],
                       in1=sigt[si][:], op=ALU.mult)
                    OP("dve", "scalar_tensor_tensor", reads=[B_acc[c], B_tt[si]], writes=[B_acc[c]], out=accT[:, c, jr],
                       in0=accT[:, c, jr], scalar=ALPHA, in1=tt[si][:], op0=ALU.mult, op1=ALU.add)
            ws_done()
        dbg_dump("acc0", accT[:], B_acc)
        p.barrier()
        if stop == "F":
            return finish()

        tmp.reset(f_mark, SB_END)
        aux.reset()
        hT = [aux.alloc([128, 8, TOK], BF16) for _ in range(2)]
        B_hT = [Buf("hT0"), Buf("hT1")]
        cbc = [tmp.alloc([128, TOK], F32) for _ in range(2)]
        B_cbc = [Buf("cbc0"), Buf("cbc1")]
        sgt = [tmp.alloc([128, 512], F32) for _ in range(2)]
        tt = [tmp.alloc([128, 512], F32) for _ in range(2)]
        B_sgt = [Buf("sgt0"), Buf("sgt1")]
        B_tt = [Buf("tt0"), Buf("tt1")]
        it = 0
        ity = 0
        for e in range(n_exp):
            hb = e % 2
            for j in range(2):
                MM(6, sel[:, e, :], combT[:, j * 512:(j + 1) * 512], True, True, [B_sel, B_combT])
                OP("act", "activation", reads=[psB[6]], writes=[B_cbc[hb]], out=cbc[hb][:, j * 512:(j + 1) * 512],
                   in_=ps[6][:], func=AF.Copy)
            for fq in range(4):
                s1, B_s1 = ws_next()
                s3, B_s3 = ws_next()
                for f2 in range(2):
                    fc = fq * 2 + f2
                    cs = slice(f2 * 128, (f2 + 1) * 128)
                    for j in range(2):
                        jr = slice(j * 512, (j + 1) * 512)
                        bg, bu, si = it % 2, 2 + it % 2, it % 2
                        it += 1
                        for k in range(16):
                            MM(bg, s1[:, k, cs], x1T[:, k, jr], k == 0, k == 15, [B_s1, B_r3])
                        for k in range(16):
                            MM(bu, s3[:, k, cs], x1T[:, k, jr], k == 0, k == 15, [B_s3, B_r3])
                        OP("act", "activation", reads=[psB[bg]], writes=[B_sgt[si]], out=sgt[si][:], in_=ps[bg][:],
                           func=AF.Silu)
                        OP("dve", "tensor_tensor", reads=[psB[bu], B_sgt[si]], writes=[B_tt[si]], out=tt[si][:],
                           in0=ps[bu][:], in1=sgt[si][:], op=ALU.mult)
                        OP("pool", "tensor_tensor", reads=[B_tt[si], B_cbc[hb]], writes=[B_hT[hb]], out=hT[hb][:, fc, jr],
                           in0=tt[si][:], in1=cbc[hb][:, jr], op=ALU.mult)
                ws_done(2)
            for cs4 in range(4):
                s2, B_s2 = ws_next()
                for cc in range(4):
                    c = cs4 * 4 + cc
                    for j in range(2):
                        jr = slice(j * 512, (j + 1) * 512)
                        by = 4 + ity % 2
                        ity += 1
                        for fc in range(8):
                            MM(by, s2[:, fc, cc * 128:(cc + 1) * 128], hT[hb][:, fc, jr], fc == 0, fc == 7, [B_s2, B_hT[hb]])
                        OP("dve", "tensor_tensor", reads=[psB[by], B_acc[c]], writes=[B_acc[c]], out=accT[:, c, jr],
                           in0=ps[by][:], in1=accT[:, c, jr], op=ALU.add)
                ws_done()
        p.barrier()

        tmp.reset(r6_lo, SB_END)
        ln_feat(V_L2G, V_L2B, None, None)
        p.barrier()
        aux.reset(r2_lo, r4_lo)
        osb = [aux.alloc([128, D], F32) for _ in range(2)]
        B_osb = [Buf("osb0"), Buf("osb1")]
        d_os = [p.dsem("osb0"), p.dsem("osb1")]
        for s8 in range(8):
            ob = s8 % 2
            for cq in range(4):
                bank = cq % 2
                for c4 in range(4):
                    c = cq * 4 + c4
                    OP("pe", "transpose", reads=[B_acc[c], B_const], writes=[psB[bank]],
                       out=ps[bank][:, c4 * 128:(c4 + 1) * 128], in_=accT[:, c, s8 * 128:(s8 + 1) * 128], identity=ident_f[:])
                if bank == 0:
                    OP("act", "activation", reads=[psB[bank]], writes=[B_osb[ob]], out=osb[ob][:, cq * 512:(cq + 1) * 512],
                       in_=ps[bank][:], func=AF.Copy)
                else:
                    OP("dve", "tensor_copy", reads=[psB[bank]], writes=[B_osb[ob]], out=osb[ob][:, cq * 512:(cq + 1) * 512],
                       in_=ps[bank][:])
            DMA("sp", d_os[ob], [(out[s8 * 128:(s8 + 1) * 128, :], osb[ob][:])], reads=[B_osb[ob]])
        return finish()


def _rope_tables():
    half = 64
    inv_freq = (1.0 / (np.float32(10000.0) ** (np.arange(0, half, 2, dtype=np.float32) / np.float32(half)))).astype(np.float32)
    tok = np.arange(S)
    row = (tok // 64).astype(np.float32)
    col = (tok % 64).astype(np.float32)
    ang_r = (row[:, None] * inv_freq[None, :]).astype(np.float32)
    ang_c = (col[:, None] * inv_freq[None, :]).astype(np.float32)
    cos = np.concatenate([np.cos(ang_r), np.cos(ang_r), np.cos(ang_c), np.cos(ang_c)], axis=1).astype(np.float32)
    sin = np.concatenate([-np.sin(ang_r), np.sin(ang_r), -np.sin(ang_c), np.sin(ang_c)], axis=1).astype(np.float32)
    return np.ascontiguousarray(cos.T), np.ascontiguousarray(sin.T)


def _pc(v):
    return np.ascontiguousarray(np.asarray(v, np.float32).reshape(16, 128).T)


def prep_inputs(inp, cores=range(NCORES)):
    f = lambda a: np.ascontiguousarray(np.asarray(a, np.float32))
    x = f(inp["x"][0])
    pfull = f(inp["p"][0, 0])
    cosT, sinT = _rope_tables()
    vecs = np.stack([_pc(inp["emb_ln_g"]), _pc(inp["emb_ln_b"]), _pc(inp["ln1_g"][0]), _pc(inp["ln1_b"][0]),
                     _pc(inp["ln2_g"][0]), _pc(inp["ln2_b"][0]), _pc(inp["b_gate"][0, :D]), _pc(inp["b_gate"][0, D:]),
                     _pc(inp["b_ple_gate"][0]), _pc(inp["conv_w"][0, 0]), _pc(inp["conv_w"][0, 1]), _pc(inp["conv_w"][0, 2])],
                    axis=1)
    vecs = np.ascontiguousarray(vecs.astype(np.float32))
    ident = np.eye(128, dtype=np.float32)
    rmat = np.zeros((128, 128), np.float32)
    for m in range(128):
        partner = m + 32 if (m % 64) < 32 else m - 32
        rmat[partner, m] = 1.0
    cmat = np.ascontiguousarray(np.stack([ident, rmat], axis=1))
    gq = f(inp["q_norm_g"][0]); gk = f(inp["k_norm_g"][0])
    gqk = np.ascontiguousarray(np.stack([gq, gk], axis=1))
    gqk_row = np.ascontiguousarray(np.broadcast_to(np.stack([gq, gk], axis=0)[None], (128, 2, 128)))
    wr = np.concatenate([f(inp["w_group_router"][0]), f(inp["w_expert_router"][0]).reshape(D, 16)], axis=1)
    wr = np.ascontiguousarray(wr.reshape(16, 128, 20).transpose(1, 0, 2))
    br = np.concatenate([f(inp["b_group_router"][0]), f(inp["b_expert_router"][0]).reshape(16)])
    br = np.ascontiguousarray(np.broadcast_to(br[None], (128, 20)))
    sel = np.zeros((16, 16, 128), np.float32)
    for e in range(16):
        sel[e, e, :] = 1.0
    shared = {
        "x_all": x, "cos_all": cosT, "sin_all": sinT,
        "w_in": f(inp["w_in"][0]), "w_ao": f(inp["w_attn_o"][0]), "w_co": f(inp["w_conv_o"][0]),
        "w_out": f(inp["w_out"][0]), "w_pg": f(inp["w_ple_gate"][0]), "w_ple": f(inp["w_ple"][0]),
        "w1": f(inp["w_exp_gate"][0]).reshape(16, D, 1024), "w3": f(inp["w_exp_up"][0]).reshape(16, D, 1024),
        "w2": f(inp["w_exp_down"][0]).reshape(16, 1024, D),
        "vecs": vecs, "cmat": cmat, "gqk": gqk, "gqk_row": gqk_row, "wr": wr, "br": br, "sel": sel,
    }
    maps = []
    for c in cores:
        lo, hi = c * TOK, (c + 1) * TOK
        halo = np.zeros((2, D), np.float32)
        hm = np.zeros((128, 2), np.float32)
        if lo > 0:
            halo[0] = x[lo - 1]; hm[:, 0] = 1.0
        if hi < S:
            halo[1] = x[hi]; hm[:, 1] = 1.0
        m = dict(shared)
        m.update({"x_own": np.ascontiguousarray(x[lo:hi]), "x_halo": halo, "hmask": hm,
                  "pT": np.ascontiguousarray(pfull[lo:hi].T),
                  "cos_own": np.ascontiguousarray(cosT[:, lo:hi]), "sin_own": np.ascontiguousarray(sinT[:, lo:hi])})
        maps.append(m)
    return maps


def kernel(**inp):
    nc = build()
    maps = prep_inputs(inp)
    res = run_bass_kernel_spmd(nc, maps, core_ids=list(range(NCORES)))
    outs = [np.asarray(r["out"], np.float32) for r in res.results]
    return np.concatenate(outs, axis=0).reshape(1, S, D)
```

```python
import os
from contextlib import ExitStack
import numpy as np
import concourse.bass as bass
import concourse.mybir as mybir
from concourse.bass_utils import run_bass_kernel_spmd

F32 = mybir.dt.float32
BF16 = mybir.dt.bfloat16
AF = mybir.ActivationFunctionType
ALU = mybir.AluOpType

NCORES = 8
S = 8192
D = 2048
TOK = 1024
TOKX = 1026
ALPHA = float(2.0 ** 0.25)
LN_EPS = 1e-5
QK_EPS = 1e-6
O_Q, O_K, O_V, O_CB, O_CC, O_CH, O_GA, O_GC = 0, 2048, 2560, 3072, 5120, 7168, 9216, 11264
SB_BASE = 16512
SB_END = 229376
NSLOT = 5
SLOT_BYTES = 8192
SCRATCH_KIND = "Internal"


class Buf:
    __slots__ = ("name", "lw", "rd", "excl")

    def __init__(self, name="", excl=False):
        self.name = name
        self.lw = None
        self.rd = {}
        self.excl = excl


class DmaSem:
    __slots__ = ("handle", "total", "name")

    def __init__(self, handle, name):
        self.handle = handle
        self.total = 0
        self.name = name


class Ins:
    __slots__ = ("eng", "fn", "deps", "sig", "sigval", "is_dma", "dsem", "dval", "ndma")

    def __init__(self, eng, fn):
        self.eng = eng
        self.fn = fn
        self.deps = {}
        self.sig = False
        self.sigval = 0
        self.is_dma = False
        self.dsem = None
        self.dval = 0
        self.ndma = 0

    def key(self):
        return ("d", id(self.dsem)) if self.is_dma else ("e", self.eng)


ENGS = ("pe", "act", "dve", "pool", "sp")


class Prog:
    def __init__(self, nc, stack):
        self.nc = nc
        self.stack = stack
        self.streams = {e: [] for e in ENGS}
        self.pending = {e: {} for e in ENGS}
        self.esem = {}
        self.dsems = []
        self.all_dma = {}
        for e in ENGS:
            if e != "sp":
                self.esem[e] = stack.enter_context(nc.semaphore("es_" + e))

    def dsem(self, name):
        h = self.stack.enter_context(self.nc.semaphore("ds_" + name))
        d = DmaSem(h, name)
        self.dsems.append(d)
        return d

    @staticmethod
    def _later(a, b):
        if a.is_dma:
            return a.dval > b.dval
        return a.sigval > b.sigval

    def _add(self, ins, reads, writes):
        deps = ins.deps
        if any(b.excl for b in reads):
            writes = list(writes) + [b for b in reads if b.excl]
            reads = [b for b in reads if not b.excl]

        def add(d):
            if d is None:
                return
            if (not d.is_dma) and (not ins.is_dma) and d.eng == "pe" and ins.eng == "pe":
                return
            k = d.key()
            o = deps.get(k)
            if o is None or self._later(d, o):
                deps[k] = d

        for b in reads:
            add(b.lw)
        for b in writes:
            add(b.lw)
            for d in b.rd.values():
                add(d)
        for d in self.pending[ins.eng].values():
            add(d)
        self.pending[ins.eng] = {}
        for d in deps.values():
            d.sig = True
        for b in reads:
            b.rd[ins.key()] = ins
        for b in writes:
            b.lw = ins
            b.rd = {}
        self.streams[ins.eng].append(ins)
        ins.sigval = len(self.streams[ins.eng])
        return ins

    def op(self, eng, fn, reads=(), writes=()):
        return self._add(Ins(eng, fn), reads, writes)

    def dma(self, eng, fn, ndma, sem, reads=(), writes=()):
        ins = Ins(eng, fn)
        ins.is_dma = True
        ins.dsem = sem
        ins.ndma = ndma
        sem.total += 16 * ndma
        ins.dval = sem.total
        self.all_dma[id(sem)] = ins
        return self._add(ins, reads, writes)

    def barrier(self):
        last = {}
        for e in ENGS:
            for i in reversed(self.streams[e]):
                if not i.is_dma:
                    last[("e", e)] = i
                    break
        for k, d in self.all_dma.items():
            last[("d", k)] = d
        for e in ENGS:
            self.pending[e] = dict(last)

    def emit(self):
        nc = self.nc
        for e in ENGS:
            c = 0
            for i in self.streams[e]:
                if i.is_dma:
                    continue
                if i.sig:
                    c += 1
                    i.sigval = c
                else:
                    i.sigval = -1
        streams = self.streams
        esem = self.esem

        def run(e, eng):
            waited = {}
            for ins in streams[e]:
                for k, d in ins.deps.items():
                    if d.is_dma:
                        sem, val = d.dsem.handle, d.dval
                    else:
                        assert d.sigval > 0
                        sem, val = esem[d.eng], d.sigval
                    if waited.get(k, 0) >= val:
                        continue
                    waited[k] = val
                    eng.wait_ge(sem, val)
                r = ins.fn(eng)
                if ins.is_dma:
                    assert len(r) == ins.ndma, (len(r), ins.ndma)
                    for x in r:
                        x.then_inc(ins.dsem.handle, 16)
                elif ins.sig:
                    r.then_inc(esem[e], 1)

        with nc.Block() as block:
            @block.tensor
            def _(eng):
                run("pe", eng)

            @block.scalar
            def _(eng):
                run("act", eng)

            @block.vector
            def _(eng):
                run("dve", eng)

            @block.gpsimd
            def _(eng):
                run("pool", eng)

            @block.sync
            def _(eng):
                run("sp", eng)
                for d in self.dsems:
                    if d.total > 0:
                        eng.wait_ge(d.handle, d.total)


class Arena:
    def __init__(self, nc, lo, hi, tag):
        self.nc, self.lo, self.hi, self.off, self.tag, self.n = nc, lo, hi, lo, tag, 0

    def reset(self, lo=None, hi=None):
        if lo is not None:
            self.lo = lo
        if hi is not None:
            self.hi = hi
        self.off = self.lo

    def alloc(self, shape, dt):
        sz = int(np.prod(shape[1:])) * mybir.dt.size(dt)
        sz = (sz + 63) // 64 * 64
        assert self.off + sz <= self.hi, ("SBUF arena overflow", self.tag, self.off, sz, self.hi)
        self.n += 1
        t = self.nc.alloc_sbuf_tensor_at("%s_%d" % (self.tag, self.n), list(shape), dt, offset=self.off)
        self.off += sz
        return t


def build(dbg=None, stop=None):
    nc = bass.Bass("TRN2", target_bir_lowering=False)

    def din(name, shape, dt=F32):
        return nc.dram_tensor(name, list(shape), dt, kind="ExternalInput").ap()

    x_all = din("x_all", [S, D])
    x_own = din("x_own", [TOK, D])
    x_halo = din("x_halo", [2, D])
    pT = din("pT", [256, TOK])
    cos_all = din("cos_all", [128, S])
    sin_all = din("sin_all", [128, S])
    cos_own = din("cos_own", [128, TOK])
    sin_own = din("sin_own", [128, TOK])
    w_in = din("w_in", [D, 13312])
    w_ao = din("w_ao", [D, D])
    w_co = din("w_co", [D, D])
    w_out = din("w_out", [D, D])
    w_pg = din("w_pg", [D, D])
    w_ple = din("w_ple", [256, D])
    w1 = din("w1", [16, D, 1024])
    w3 = din("w3", [16, D, 1024])
    w2 = din("w2", [16, 1024, D])
    vecs_d = din("vecs", [128, 12, 16])
    cmat_d = din("cmat", [128, 2, 128])
    gqk_d = din("gqk", [128, 2])
    gqk_row_d = din("gqk_row", [128, 2, 128])
    wr_d = din("wr", [128, 16, 20])
    br_d = din("br", [128, 20])
    sel_d = din("sel", [16, 16, 128])
    hmask_d = din("hmask", [128, 2])
    out = nc.dram_tensor("out", [TOK, D], F32, kind="ExternalOutput").ap()
    kT_s = nc.dram_tensor("kT_s", [4, 128, S], BF16, kind=SCRATCH_KIND).ap()
    v_s = nc.dram_tensor("v_s", [S, 512], BF16, kind=SCRATCH_KIND).ap()
    xres_s = nc.dram_tensor("xres_s", [128, 16, TOK], F32, kind=SCRATCH_KIND).ap()
    dbg_out = {}
    if dbg:
        for name, shape, dt in dbg:
            dbg_out[name] = nc.dram_tensor("dbg_" + name, list(shape), dt, kind="ExternalOutput").ap()

    st = ExitStack()
    with st:
        p = Prog(nc, st)

        def OP(eng, meth, reads=(), writes=(), **kw):
            p.op(eng, lambda e: getattr(e, meth)(**kw), reads, writes)

        def DMA(eng, sem, pairs, reads=(), writes=()):
            pairs = list(pairs)
            p.dma(eng, lambda e: [e.dma_start(out=o, in_=i) for o, i in pairs], len(pairs), sem, reads, writes)

        def MM(bank, lhsT, rhs, start, stop_, reads, cols=None):
            o = ps[bank][:] if cols is None else ps[bank][:, cols[0]:cols[1]]
            OP("pe", "matmul", reads=reads, writes=[psB[bank]], out=o, lhsT=lhsT, rhs=rhs, start=start, stop=stop_)

        def finish():
            p.emit()
            return nc

        def dbg_dump(name, src_ap, reads):
            if name in dbg_out:
                DMA("sp", p.dsem("dbg_" + name), [(dbg_out[name], src_ap)], reads=reads)

        off = SB_BASE
        misc = Arena(nc, off, off + 3584, "misc"); off += 3584
        slots_lo = off
        off += NSLOT * SLOT_BYTES
        q_lo = off
        off += 8192
        r2_lo = off
        off += 32896
        r3_lo = off; off += 32768
        r4_lo = off; off += 32768
        r5_lo = off; off += 32768
        r6_lo = off
        tmp = Arena(nc, r6_lo, SB_END, "tmp")
        aux = Arena(nc, r2_lo, r3_lo, "aux")

        ident_f = misc.alloc([128, 128], F32)
        rmat_f = misc.alloc([128, 128], F32)
        ident_b = misc.alloc([128, 128], BF16)
        ones_b = misc.alloc([128, 128], BF16)
        ones_f = misc.alloc([128, 128], F32)
        vecs = misc.alloc([128, 12, 16], F32)
        gqk = misc.alloc([128, 2], F32)
        negc = misc.alloc([128, 1], F32)
        eps_ln = misc.alloc([128, 1], F32)
        eps_qk = misc.alloc([128, 1], F32)
        hmask = misc.alloc([128, 2], F32)
        V_EG, V_EB, V_L1G, V_L1B, V_L2G, V_L2B, V_BGA, V_BGC, V_BPG, V_CW0, V_CW1, V_CW2 = range(12)
        B_const = Buf("const")
        xT_own = nc.alloc_sbuf_tensor_at("xT_own", [128, 16, TOKX], BF16, offset=r2_lo)
        B_xT = Buf("xT_own")
        sT = nc.alloc_sbuf_tensor_at("sT", [128, 16, TOK], BF16, offset=r3_lo)
        x1T = sT
        B_r3 = Buf("r3")
        gqaT = nc.alloc_sbuf_tensor_at("gqaT", [128, 16, TOK], BF16, offset=r4_lo)
        convinT = gqaT
        B_r4 = Buf("r4")
        kT_g = nc.alloc_sbuf_tensor_at("kT_g", [128, S], BF16, offset=r5_lo)
        v_g = nc.alloc_sbuf_tensor_at("v_g", [128, 64, 128], BF16, offset=r5_lo + 16384)
        B_kv = Buf("kv")
        accT = nc.alloc_sbuf_tensor_at("accT", [128, 16, TOK], F32, offset=r4_lo)
        B_acc = [Buf("acc%d" % c) for c in range(16)]
        slots = [nc.alloc_sbuf_tensor_at("slot%d" % i, [128, SLOT_BYTES // 2], BF16, offset=slots_lo + i * SLOT_BYTES)
                 for i in range(NSLOT)]
        B_slot = [Buf("slot%d" % i) for i in range(NSLOT)]

        ps = [nc.alloc_psum_tensor("ps%d" % i, [128, 512], F32) for i in range(8)]
        psB = [Buf("ps%d" % i, excl=True) for i in range(8)]

        d_c = p.dsem("const")
        d_x = [p.dsem("x%d" % i) for i in range(4)]
        d_o = p.dsem("store")
        d_t = [p.dsem("tbl%d" % i) for i in range(2)]
        d_kv = p.dsem("kv")
        d_slot = [p.dsem("slot%d" % i) for i in range(NSLOT)]

        def vcol(i, c):
            return vecs[:, i, c:c + 1]

        DMA("sp", d_c, [(ident_f[:], cmat_d[:, 0, :]), (rmat_f[:], cmat_d[:, 1, :]), (vecs[:], vecs_d),
                        (gqk[:], gqk_d), (hmask[:], hmask_d)], writes=[B_const])
        OP("dve", "tensor_copy", reads=[B_const], writes=[B_const], out=ident_b[:], in_=ident_f[:])
        OP("dve", "memset", writes=[B_const], ap=ones_b[:], constant=1.0 / 128.0)
        OP("dve", "memset", writes=[B_const], ap=ones_f[:], constant=1.0 / 2048.0)
        OP("dve", "memset", writes=[B_const], ap=eps_ln[:], constant=LN_EPS)
        OP("dve", "memset", writes=[B_const], ap=eps_qk[:], constant=QK_EPS)
        tmp.reset(r3_lo, SB_END)
        grow = tmp.alloc([128, 2, 128], F32)
        gmx = tmp.alloc([128, 2], F32)
        B_g = Buf("grow")
        DMA("sp", p.dsem("grow"), [(grow[:], gqk_row_d)], writes=[B_g])
        OP("dve", "tensor_reduce", reads=[B_g], writes=[B_g], out=gmx[:], in_=grow[:], axis=mybir.AxisListType.X,
           op=ALU.max, apply_absolute_value=True)
        OP("dve", "scalar_tensor_tensor", reads=[B_g], writes=[B_const], out=negc[:], in0=gmx[:, 0:1],
           scalar=-float(np.sqrt(128.0)), in1=gmx[:, 1:2], op0=ALU.mult, op1=ALU.mult)
        p.barrier()

        def ln_tile(xin_t, B_x, z_t, B_z, T, npart=128, lnexp=False):
            st6, mv, rs, nm, B_t = T
            for q in range(4):
                OP("dve", "bn_stats", reads=[B_x], writes=[B_t], out=st6[0:npart, q, :],
                   in_=xin_t[0:npart, q * 512:(q + 1) * 512])
            OP("dve", "bn_aggr", reads=[B_t], writes=[B_t], out=mv[0:npart, :], in_=st6[0:npart, :, :])
            if lnexp:
                OP("act", "activation", reads=[B_t, B_const], writes=[B_t], out=rs[0:npart, :], in_=mv[0:npart, 1:2],
                   func=AF.Ln, bias=eps_ln[0:npart, :], scale=1.0)
                OP("act", "activation", reads=[B_t], writes=[B_t], out=rs[0:npart, :], in_=rs[0:npart, :],
                   func=AF.Exp, scale=-0.5)
            else:
                OP("act", "activation", reads=[B_t, B_const], writes=[B_t], out=rs[0:npart, :], in_=mv[0:npart, 1:2],
                   func=AF.Sqrt, bias=eps_ln[0:npart, :], scale=1.0)
                OP("dve", "reciprocal", reads=[B_t], writes=[B_t], out=rs[0:npart, :], in_=rs[0:npart, :])
            OP("dve", "scalar_tensor_tensor", reads=[B_t], writes=[B_t], out=nm[0:npart, :], in0=mv[0:npart, 0:1],
               scalar=-1.0, in1=rs[0:npart, :], op0=ALU.mult, op1=ALU.mult)
            OP("act", "activation", reads=[B_x, B_t], writes=[B_z], out=z_t[0:npart, :], in_=xin_t[0:npart, :],
               func=AF.Identity, bias=nm[0:npart, :], scale=rs[0:npart, :])

        def ln_temps(n):
            return [(tmp.alloc([128, 4, 6], F32), tmp.alloc([128, 2], F32), tmp.alloc([128, 1], F32),
                     tmp.alloc([128, 1], F32), Buf("lnt%d" % i)) for i in range(n)]

        def rms_rope(bank, gcol, cos_ap, sin_ap, B_tab, out_ap, B_out, T, psx0, psx1):
            sq, rt, kn, B_sq, B_rt, B_kn = T
            src = ps[bank][:]
            OP("act", "activation", reads=[psB[bank]], writes=[B_sq], out=sq[:], in_=src, func=AF.Square)
            MM(psx0, ones_b[:], sq[:], True, True, [B_sq, B_const])
            OP("act", "activation", reads=[psB[psx0], B_const], writes=[B_rt], out=rt[:], in_=ps[psx0][:], func=AF.Sqrt,
               bias=eps_qk[:], scale=1.0)
            OP("dve", "reciprocal", reads=[B_rt], writes=[B_rt], out=rt[:], in_=rt[:])
            OP("dve", "scalar_tensor_tensor", reads=[psB[bank], B_rt, B_const], writes=[B_kn], out=kn[:], in0=src,
               scalar=gcol, in1=rt[:], op0=ALU.mult, op1=ALU.mult)
            MM(psx1, rmat_f[:], kn[:], True, True, [B_kn, B_const])
            OP("dve", "tensor_tensor", reads=[psB[psx1], B_tab], writes=[B_rt], out=rt[:], in0=ps[psx1][:], in1=sin_ap,
               op=ALU.mult)
            OP("pool", "tensor_tensor", reads=[B_kn, B_tab], writes=[B_kn], out=kn[:], in0=kn[:], in1=cos_ap, op=ALU.mult)
            OP("pool", "tensor_tensor", reads=[B_kn, B_rt], writes=[B_out], out=out_ap, in0=kn[:], in1=rt[:], op=ALU.add)

        def chain_temps(n):
            return [(tmp.alloc([128, 512], BF16), tmp.alloc([128, 512], F32), tmp.alloc([128, 512], F32),
                     Buf("sq%d" % i), Buf("rt%d" % i), Buf("kn%d" % i)) for i in range(n)]

        tmp.reset(r3_lo, SB_END)
        xin = [tmp.alloc([128, D], F32) for _ in range(4)]
        B_xin = [Buf("xin%d" % i) for i in range(4)]
        zf = [tmp.alloc([128, D], F32) for _ in range(4)]
        B_zf = [Buf("zf%d" % i) for i in range(4)]
        stage = tmp.alloc([128, 16, 512], F32)
        B_stage = Buf("stage")
        lnT = ln_temps(4)
        for t in range(2):
            for s4 in range(4):
                sub = t * 4 + s4
                DMA("sp", d_x[s4], [(xin[s4][:], x_own[sub * 128:(sub + 1) * 128, :])], writes=[B_xin[s4]])
                ln_tile(xin[s4], B_xin[s4], zf[s4], B_zf[s4], lnT[s4])
            for c in range(16):
                pb = c % 2
                for s4 in range(4):
                    OP("pe", "transpose", reads=[B_zf[s4], B_const], writes=[psB[pb]],
                       out=ps[pb][:, s4 * 128:(s4 + 1) * 128], in_=zf[s4][:, c * 128:(c + 1) * 128], identity=ident_f[:])
                OP("dve", "tensor_scalar", reads=[psB[pb], B_const], writes=[B_stage], out=stage[:, c, :], in0=ps[pb][:],
                   scalar1=vcol(V_EG, c), scalar2=vcol(V_EB, c), op0=ALU.mult, op1=ALU.add)
                OP("act", "activation", reads=[B_stage], writes=[B_xT], out=xT_own[:, c, t * 512:(t + 1) * 512],
                   in_=stage[:, c, :], func=AF.Copy)
            DMA("sp", d_o, [(xres_s[:, :, t * 512:(t + 1) * 512], stage[:])], reads=[B_stage])
        DMA("sp", d_x[0], [(xin[0][0:2, :], x_halo)], writes=[B_xin[0]])
        ln_tile(xin[0], B_xin[0], zf[0], B_zf[0], lnT[0], npart=2)
        for c in range(16):
            OP("pe", "transpose", reads=[B_zf[0], B_const], writes=[psB[2]], out=ps[2][:, c * 2:(c + 1) * 2],
               in_=zf[0][0:2, c * 128:(c + 1) * 128], identity=ident_f[0:2, 0:2])
        htmp = tmp.alloc([128, 16, 2], F32)
        B_ht = Buf("htmp")
        OP("dve", "tensor_tensor", reads=[psB[2], B_const], writes=[B_ht], out=htmp[:],
           in0=ps[2][:, 0:32].rearrange("p (c t) -> p c t", t=2),
           in1=vecs[:, V_EG, :].unsqueeze(2).broadcast_to([128, 16, 2]), op=ALU.mult)
        OP("dve", "tensor_tensor", reads=[B_ht, B_const], writes=[B_xT], out=xT_own[:, :, TOK:TOKX], in0=htmp[:],
           in1=vecs[:, V_EB, :].unsqueeze(2).broadcast_to([128, 16, 2]), op=ALU.add)
        dbg_dump("xT_own", xT_own[:], [B_xT])
        p.barrier()
        if stop == "B":
            return finish()

        tmp.reset(slots_lo, r2_lo)
        wkv = tmp.alloc([128, 16, 512], BF16)
        wkv2 = tmp.alloc([128, 16, 512], BF16)
        B_wkv = Buf("wkv")
        tmp.reset(r3_lo, SB_END)
        xTt = [tmp.alloc([128, 16, 512], BF16) for _ in range(2)]
        B_xTt = [Buf("xTt0"), Buf("xTt1")]
        xinA = [tmp.alloc([128, D], F32) for _ in range(4)]
        zb = [tmp.alloc([128, D], BF16) for _ in range(8)]
        B_zb = [Buf("zb%d" % i) for i in range(8)]
        rope = [(tmp.alloc([128, 512], F32), tmp.alloc([128, 512], F32)) for _ in range(2)]
        B_rope = [Buf("rope0"), Buf("rope1")]
        cT = chain_temps(2)
        kout = [tmp.alloc([128, 512], BF16) for _ in range(2)]
        B_kout = [Buf("kout0"), Buf("kout1")]
        vout = [tmp.alloc([128, 512], BF16) for _ in range(2)]
        B_vout = [Buf("vout0"), Buf("vout1")]
        lnTA = ln_temps(4)
        d_ko = [p.dsem("kout0"), p.dsem("kout1")]
        d_vo = [p.dsem("vout0"), p.dsem("vout1")]
        w_in_v = w_in.rearrange("(c p) n -> p c n", p=128)
        DMA("pool", p.dsem("wkv"), [(wkv[:], w_in_v[:, :, O_K:O_K + 512]), (wkv2[:], w_in_v[:, :, O_V:O_V + 512])],
            writes=[B_wkv])
        NT = 1 if stop == "A1" else 16

        def A_ldma_x(t):
            for s4 in range(4):
                sub = t * 4 + s4
                DMA("sp", d_x[s4], [(xinA[s4][:], x_all[sub * 128:(sub + 1) * 128, :])], writes=[B_xin[s4]])

        def A_ldma_rope(t):
            tp = t % 2
            DMA("sp", d_t[tp], [(rope[tp][0][:], cos_all[:, t * 512:(t + 1) * 512]),
                                (rope[tp][1][:], sin_all[:, t * 512:(t + 1) * 512])], writes=[B_rope[tp]])

        def A_ln(t, s4):
            zi = (t % 2) * 4 + s4
            ln_tile(xinA[s4], B_xin[s4], zb[zi], B_zb[zi], lnTA[s4], lnexp=True)

        def A_T(t, c0, c1):
            tp = t % 2
            for c in range(c0, c1):
                pb = c % 2
                pv = ps[pb][:].bitcast(BF16)
                for s4 in range(4):
                    zi = tp * 4 + s4
                    OP("pe", "transpose", reads=[B_zb[zi], B_const], writes=[psB[pb]], out=pv[:, s4 * 128:(s4 + 1) * 128],
                       in_=zb[zi][:, c * 128:(c + 1) * 128], identity=ident_b[:])
                OP("act", "activation", reads=[psB[pb], B_const], writes=[B_xTt[tp]], out=xTt[tp][:, c, :],
                   in_=pv[:, 0:512], func=AF.Identity, bias=vcol(V_EB, c), scale=vcol(V_EG, c))

        def A_fin(t, h):
            hb, tp = h % 2, t % 2
            sq, rt, kn, B_sq, B_rt, B_kn = cT[hb]
            MM(5, rmat_f[:], kn[:], True, True, [B_kn, B_const])
            OP("dve", "tensor_tensor", reads=[psB[5], B_rope[tp]], writes=[B_rt], out=rt[:], in0=ps[5][:],
               in1=rope[tp][1][:], op=ALU.mult)
            OP("pool", "tensor_tensor", reads=[B_kn, B_rope[tp]], writes=[B_kn], out=kn[:], in0=kn[:], in1=rope[tp][0][:],
               op=ALU.mult)
            OP("pool", "tensor_tensor", reads=[B_kn, B_rt], writes=[B_kout[hb]], out=kout[hb][:], in0=kn[:], in1=rt[:],
               op=ALU.add)
            DMA("pool", d_ko[hb], [(kT_s[h, :, t * 512:(t + 1) * 512], kout[hb][:])], reads=[B_kout[hb]])

        def A_KV(t, h):
            hb, tp = h % 2, t % 2
            sq, rt, kn, B_sq, B_rt, B_kn = cT[hb]
            for c in range(16):
                MM(2 + hb, wkv[:, c, h * 128:(h + 1) * 128], xTt[tp][:, c, :], c == 0, c == 15, [B_wkv, B_xTt[tp]])
            OP("act", "activation", reads=[psB[2 + hb]], writes=[B_sq], out=sq[:], in_=ps[2 + hb][:], func=AF.Square)
            for c in range(16):
                MM(6 + hb, xTt[tp][:, c, h * 128:(h + 1) * 128], wkv2[:, c, :], c == 0, c == 15, [B_wkv, B_xTt[tp]])
            MM(4, ones_b[:], sq[:], True, True, [B_sq, B_const])
            OP("act", "activation", reads=[psB[6 + hb]], writes=[B_vout[hb]], out=vout[hb][:], in_=ps[6 + hb][:], func=AF.Copy)
            r0 = (t * 4 + h) * 128
            DMA("pool", d_vo[hb], [(v_s[r0:r0 + 128, :], vout[hb][:])], reads=[B_vout[hb]])
            OP("act", "activation", reads=[psB[4], B_const], writes=[B_rt], out=rt[:], in_=ps[4][:], func=AF.Ln,
               bias=eps_qk[:], scale=1.0)
            OP("act", "activation", reads=[B_rt], writes=[B_rt], out=rt[:], in_=rt[:], func=AF.Exp, scale=-0.5)
            if h > 0:
                A_fin(t, h - 1)
            OP("dve", "scalar_tensor_tensor", reads=[psB[2 + hb], B_rt, B_const], writes=[B_kn], out=kn[:], in0=ps[2 + hb][:],
               scalar=gqk[:, 1:2], in1=rt[:], op0=ALU.mult, op1=ALU.mult)

        for i in range(-2, NT):
            if 0 <= i + 1 < NT:
                A_ldma_rope(i + 1)
            if 0 <= i + 2 < NT:
                A_ldma_x(i + 2)
            for h in range(4):
                if 0 <= i < NT:
                    A_KV(i, h)
                if 0 <= i + 1 < NT:
                    A_T(i + 1, 4 * h, 4 * h + 4)
                if 0 <= i + 2 < NT:
                    A_ln(i + 2, h)
            if 0 <= i < NT:
                A_fin(i, 3)
        p.barrier()
        if "kT0" in dbg_out:
            DMA("sp", p.dsem("dbg2"), [(dbg_out["kT0"], kT_s[:, :, 0:512]), (dbg_out["v0"], v_s[0:512, :])])
        if stop in ("A", "A1"):
            return finish()

        def wcols(w, col0, n=256):
            return (w.rearrange("(c p) n -> p c n", p=128)[:, :, col0:col0 + n], 16, n)

        specs = []
        for g in range(4):
            for sl in range(2):
                specs.append(wcols(w_in, O_Q + g * 512 + sl * 256))
        for sg in range(8):
            specs.append(wcols(w_ao, sg * 256)); specs.append(wcols(w_in, O_GA + sg * 256))
        for sg in range(8):
            specs.append(wcols(w_in, O_CB + sg * 256)); specs.append(wcols(w_in, O_CC + sg * 256))
            specs.append(wcols(w_in, O_CH + sg * 256))
        for sg in range(8):
            specs.append(wcols(w_co, sg * 256)); specs.append(wcols(w_in, O_GC + sg * 256))
        for sg in range(8):
            specs.append(wcols(w_out, sg * 256))
        for sg in range(8):
            specs.append(wcols(w_pg, sg * 256))
        n_exp = int(os.environ.get("K_NEXP", "16"))
        for e in range(n_exp):
            for fq in range(4):
                specs.append(wcols(w1[e], fq * 256)); specs.append(wcols(w3[e], fq * 256))
            for cs4 in range(4):
                specs.append((w2[e].rearrange("(fc p) n -> p fc n", p=128)[:, :, cs4 * 512:(cs4 + 1) * 512], 8, 512))

        class WS:
            issued = 0
            nxt = 0

        def ws_issue():
            k = WS.issued
            src, a, b = specs[k]
            s_ = k % NSLOT
            dst = slots[s_][:, 0:a * b].rearrange("p (a b) -> p a b", a=a)
            DMA("pool", d_slot[s_], [(dst, src)], writes=[B_slot[s_]])
            WS.issued += 1

        def ws_next():
            k = WS.nxt
            WS.nxt += 1
            assert k < WS.issued, "weight stream underflow"
            src, a, b = specs[k]
            s_ = k % NSLOT
            return slots[s_][:, 0:a * b].rearrange("p (a b) -> p a b", a=a), B_slot[s_]

        def ws_done(n=1):
            for _ in range(n):
                if WS.issued < len(specs):
                    ws_issue()

        for _ in range(NSLOT):
            ws_issue()

        tmp.reset(r6_lo, SB_END)
        qT_g = nc.alloc_sbuf_tensor_at("qT_g", [128, 4, TOK], BF16, offset=q_lo)
        B_q = [Buf("q%d" % i) for i in range(4)]
        tabq = [(tmp.alloc([128, 512], F32), tmp.alloc([128, 512], F32)) for _ in range(2)]
        B_tabq = Buf("tabq")
        cTq = chain_temps(2)
        NPT = 5
        pt = [tmp.alloc([128, 512], BF16) for _ in range(NPT)]
        B_pt = [Buf("pt%d" % i) for i in range(NPT)]
        NS2 = 3
        s2 = [tmp.alloc([128, 512], BF16) for _ in range(NS2)]
        B_s2 = [Buf("s2_%d" % i) for i in range(NS2)]
        rec = tmp.alloc([128, 512], F32)
        B_rec = Buf("rec")
        scale = float(128.0 ** -0.5)
        n_grp = int(os.environ.get("K_NGRP", "4"))
        it = 0
        DMA("sp", d_t[0], [(tabq[j][0][:], cos_own[:, j * 512:(j + 1) * 512]) for j in range(2)] +
            [(tabq[j][1][:], sin_own[:, j * 512:(j + 1) * 512]) for j in range(2)], writes=[B_tabq])
        for g in range(n_grp):
            DMA("sp", d_kv, [(kT_g[:], kT_s[g]),
                             (v_g[:], v_s.rearrange("(kc p) (g d) -> p kc g d", p=128, g=4)[:, :, g, :])], writes=[B_kv])
            wq_cur = [None, None]

            def Q_mm(i):
                hl, j = i // 2, i % 2
                if i % 4 == 0:
                    wq_cur[0], wq_cur[1] = ws_next()
                wq, B_wq = wq_cur
                hh = hl % 2
                sq, rt, kn, B_sq, B_rt, B_kn = cTq[i % 2]
                for c in range(16):
                    MM(i % 2, wq[:, c, hh * 128:(hh + 1) * 128], xT_own[:, c, j * 512:(j + 1) * 512], c == 0, c == 15,
                       [B_wq, B_xT])
                OP("act", "activation", reads=[psB[i % 2]], writes=[B_sq], out=sq[:], in_=ps[i % 2][:], func=AF.Square)
                if i % 4 == 3:
                    ws_done()

            def Q_fin(i):
                hl, j = i // 2, i % 2
                sq, rt, kn, B_sq, B_rt, B_kn = cTq[i % 2]
                MM(7, rmat_f[:], kn[:], True, True, [B_kn, B_const])
                OP("dve", "tensor_tensor", reads=[psB[7], B_tabq], writes=[B_rt], out=rt[:], in0=ps[7][:], in1=tabq[j][1][:],
                   op=ALU.mult)
                OP("pool", "tensor_tensor", reads=[B_kn, B_tabq], writes=[B_kn], out=kn[:], in0=kn[:], in1=tabq[j][0][:],
                   op=ALU.mult)
                OP("pool", "tensor_tensor", reads=[B_kn, B_rt], writes=[B_q[hl]], out=qT_g[:, hl, j * 512:(j + 1) * 512],
                   in0=kn[:], in1=rt[:], op=ALU.add)

            Q_mm(0)
            for i in range(8):
                sq, rt, kn, B_sq, B_rt, B_kn = cTq[i % 2]
                if i + 1 < 8:
                    Q_mm(i + 1)
                MM(2, ones_b[:], sq[:], True, True, [B_sq, B_const])
                OP("act", "activation", reads=[psB[2], B_const], writes=[B_rt], out=rt[:], in_=ps[2][:], func=AF.Ln,
                   bias=eps_qk[:], scale=1.0)
                OP("act", "activation", reads=[B_rt], writes=[B_rt], out=rt[:], in_=rt[:], func=AF.Exp, scale=-0.5)
                if i > 0:
                    Q_fin(i - 1)
                OP("dve", "scalar_tensor_tensor", reads=[psB[i % 2], B_rt, B_const], writes=[B_kn], out=kn[:], in0=ps[i % 2][:],
                   scalar=gqk[:, 0:1], in1=rt[:], op0=ALU.mult, op1=ALU.mult)
            Q_fin(7)
            for hl in range(4):
                h = g * 4 + hl
                for j in range(2):
                    po = 3 + it % 2
                    pd = 5 + it % 2
                    it += 1
                    qv = qT_g[:, hl, j * 512:(j + 1) * 512]

                    def qk(kc):
                        MM(kc % 3, kT_g[:, kc * 128:(kc + 1) * 128], qv, True, True, [B_kv, B_q[hl]])

                    qk(0)
                    qk(1)
                    for kc in range(64):
                        b3 = kc % 3
                        pi = kc % NPT
                        OP("act", "activation", reads=[psB[b3], B_const], writes=[B_pt[pi]], out=pt[pi][:], in_=ps[b3][:],
                           func=AF.Exp, bias=negc[:], scale=scale)
                        if kc + 2 < 64:
                            qk(kc + 2)
                        MM(po, v_g[:, kc, :], pt[pi][:], kc == 0, kc == 63, [B_kv, B_pt[pi]])
                        if kc % 2 == 1:
                            pr = kc // 2
                            si = pr % NS2
                            pj = (kc - 1) % NPT
                            OP("dve", "tensor_tensor", reads=[B_pt[pj], B_pt[pi]], writes=[B_s2[si]], out=s2[si][:],
                               in0=pt[pj][:], in1=pt[pi][:], op=ALU.add)
                            if pr >= 1:
                                sj = (pr - 1) % NS2
                                MM(pd, ones_b[:], s2[sj][:], pr == 1, False, [B_const, B_s2[sj]])
                    MM(pd, ones_b[:], s2[31 % NS2][:], False, True, [B_const, B_s2[31 % NS2]])
                    OP("dve", "reciprocal", reads=[psB[pd]], writes=[B_rec], out=rec[:], in_=ps[pd][:])
                    OP("dve", "scalar_tensor_tensor", reads=[psB[po], B_rec], writes=[B_r4], out=gqaT[:, h, j * 512:(j + 1) * 512],
                       in0=ps[po][:], scalar=1.0 / 128.0, in1=rec[:], op0=ALU.mult, op1=ALU.mult)
        dbg_dump("gqaT", gqaT[:], [B_r4])
        p.barrier()
        if stop == "D":
            return finish()

        tmp.reset(r6_lo, SB_END)
        sigt = [tmp.alloc([128, 512], F32) for _ in range(2)]
        B_sig = [Buf("sig0"), Buf("sig1")]
        it = 0
        for sg in range(8):
            sa, B_sa = ws_next()
            sgl, B_sgl = ws_next()
            for cc2 in range(2):
                c = sg * 2 + cc2
                cs = slice(cc2 * 128, (cc2 + 1) * 128)
                for j in range(2):
                    jr = slice(j * 512, (j + 1) * 512)
                    ba, bg, si = it % 2, 2 + it % 2, it % 2
                    it += 1
                    for k in range(16):
                        MM(ba, sa[:, k, cs], gqaT[:, k, jr], k == 0, k == 15, [B_sa, B_r4])
                    for k in range(16):
                        MM(bg, sgl[:, k, cs], xT_own[:, k, jr], k == 0, k == 15, [B_sgl, B_xT])
                    OP("act", "activation", reads=[psB[bg], B_const], writes=[B_sig[si]], out=sigt[si][:], in_=ps[bg][:],
                       func=AF.Sigmoid, bias=vcol(V_BGA, c), scale=1.0)
                    OP("dve", "tensor_tensor", reads=[psB[ba], B_sig[si]], writes=[B_r3], out=sT[:, c, jr], in0=ps[ba][:],
                       in1=sigt[si][:], op=ALU.mult)
            ws_done(2)
        p.barrier()

        tmp.reset(r6_lo, SB_END)
        ccs = tmp.alloc([128, TOK], F32)
        uext = tmp.alloc([128, TOKX], F32)
        c1 = tmp.alloc([128, TOK], F32)
        c2 = tmp.alloc([128, TOK], F32)
        hcp = tmp.alloc([128, 4], F32)
        uh = tmp.alloc([128, 2], F32)
        B_ccs, B_u, B_c1, B_c2, B_h = Buf("ccs"), Buf("uext"), Buf("c1"), Buf("c2"), Buf("hcp")
        hr = slice(TOK, TOKX)
        for sg in range(8):
            scb, B_scb = ws_next()
            scc, B_scc = ws_next()
            sch, B_sch = ws_next()
            for cc2 in range(2):
                c = sg * 2 + cc2
                cs = slice(cc2 * 128, (cc2 + 1) * 128)
                for j in range(2):
                    for k in range(16):
                        MM(j, scc[:, k, cs], xT_own[:, k, j * 512:(j + 1) * 512], k == 0, k == 15, [B_scc, B_xT])
                for k in range(16):
                    MM(6, scc[:, k, cs], xT_own[:, k, hr], k == 0, k == 15, [B_scc, B_xT], cols=(0, 2))
                for j in range(2):
                    for k in range(16):
                        MM(2 + j, sch[:, k, cs], xT_own[:, k, j * 512:(j + 1) * 512], k == 0, k == 15, [B_sch, B_xT])
                for k in range(16):
                    MM(6, sch[:, k, cs], xT_own[:, k, hr], k == 0, k == 15, [B_sch, B_xT], cols=(2, 4))
                for j in range(2):
                    for k in range(16):
                        MM(4 + j, scb[:, k, cs], xT_own[:, k, j * 512:(j + 1) * 512], k == 0, k == 15, [B_scb, B_xT])
                for j in range(2):
                    OP("act", "activation", reads=[psB[j]], writes=[B_ccs], out=ccs[:, j * 512:(j + 1) * 512], in_=ps[j][:],
                       func=AF.Copy)
                for j in range(2):
                    OP("dve", "tensor_tensor", reads=[psB[2 + j], B_ccs], writes=[B_u], out=uext[:, 1 + j * 512:1 + (j + 1) * 512],
                       in0=ps[2 + j][:], in1=ccs[:, j * 512:(j + 1) * 512], op=ALU.mult)
                OP("dve", "tensor_copy", reads=[psB[6]], writes=[B_h], out=hcp[:], in_=ps[6][:, 0:4])
                OP("dve", "tensor_tensor", reads=[B_h], writes=[B_h], out=uh[:], in0=hcp[:, 0:2], in1=hcp[:, 2:4], op=ALU.mult)
                OP("dve", "tensor_tensor", reads=[B_h, B_const], writes=[B_u], out=uext[:, 0:1], in0=uh[:, 0:1],
                   in1=hmask[:, 0:1], op=ALU.mult)
                OP("dve", "tensor_tensor", reads=[B_h, B_const], writes=[B_u], out=uext[:, TOK + 1:TOK + 2], in0=uh[:, 1:2],
                   in1=hmask[:, 1:2], op=ALU.mult)
                OP("act", "activation", reads=[B_u, B_const], writes=[B_c1], out=c1[:], in_=uext[:, 1:TOK + 1],
                   func=AF.Identity, scale=vcol(V_CW1, c))
                OP("dve", "scalar_tensor_tensor", reads=[B_u, B_c1, B_const], writes=[B_c2], out=c2[:], in0=uext[:, 0:TOK],
                   scalar=vcol(V_CW0, c), in1=c1[:], op0=ALU.mult, op1=ALU.add)
                OP("dve", "scalar_tensor_tensor", reads=[B_u, B_c2, B_const], writes=[B_c1], out=c1[:], in0=uext[:, 2:TOK + 2],
                   scalar=vcol(V_CW2, c), in1=c2[:], op0=ALU.mult, op1=ALU.add)
                for j in range(2):
                    OP("dve", "tensor_tensor", reads=[psB[4 + j], B_c1], writes=[B_r4], out=convinT[:, c, j * 512:(j + 1) * 512],
                       in0=ps[4 + j][:], in1=c1[:, j * 512:(j + 1) * 512], op=ALU.mult)
            ws_done(3)
        dbg_dump("convinT", convinT[:], [B_r4])
        p.barrier()
        if stop == "E1":
            return finish()

        tmp.reset(r6_lo, SB_END)
        sigt = [tmp.alloc([128, 512], F32) for _ in range(2)]
        tt = [tmp.alloc([128, 512], F32) for _ in range(2)]
        B_sig = [Buf("sig0"), Buf("sig1")]
        B_tt = [Buf("tt0"), Buf("tt1")]
        it = 0
        for sg in range(8):
            sco, B_sco = ws_next()
            sgc, B_sgc = ws_next()
            for cc2 in range(2):
                c = sg * 2 + cc2
                cs = slice(cc2 * 128, (cc2 + 1) * 128)
                for j in range(2):
                    jr = slice(j * 512, (j + 1) * 512)
                    ba, bg, si = it % 2, 2 + it % 2, it % 2
                    it += 1
                    for k in range(16):
                        MM(ba, sco[:, k, cs], convinT[:, k, jr], k == 0, k == 15, [B_sco, B_r4])
                    for k in range(16):
                        MM(bg, sgc[:, k, cs], xT_own[:, k, jr], k == 0, k == 15, [B_sgc, B_xT])
                    OP("act", "activation", reads=[psB[bg], B_const], writes=[B_sig[si]], out=sigt[si][:], in_=ps[bg][:],
                       func=AF.Sigmoid, bias=vcol(V_BGC, c), scale=1.0)
                    OP("dve", "tensor_tensor", reads=[psB[ba], B_sig[si]], writes=[B_tt[si]], out=tt[si][:], in0=ps[ba][:],
                       in1=sigt[si][:], op=ALU.mult)
                    OP("pool", "tensor_tensor", reads=[B_tt[si], B_r3], writes=[B_r3], out=sT[:, c, jr], in0=tt[si][:],
                       in1=sT[:, c, jr], op=ALU.add)
            ws_done(2)
        dbg_dump("sT", sT[:], [B_r3])
        p.barrier()

        tmp.reset(r6_lo, SB_END)
        xres = [tmp.alloc([128, TOK], F32) for _ in range(2)]
        B_xres = [Buf("xres0"), Buf("xres1")]
        d_xr = [p.dsem("xres0"), p.dsem("xres1")]
        it = 0
        for sg in range(8):
            so, B_so = ws_next()
            for cc2 in range(2):
                c = sg * 2 + cc2
                cs = slice(cc2 * 128, (cc2 + 1) * 128)
                xr = c % 2
                DMA("sp", d_xr[xr], [(xres[xr][:], xres_s[:, c, :])], writes=[B_xres[xr]])
                for j in range(2):
                    jr = slice(j * 512, (j + 1) * 512)
                    bm = it % 2
                    it += 1
                    for k in range(16):
                        MM(bm, so[:, k, cs], sT[:, k, jr], k == 0, k == 15, [B_so, B_r3])
                    OP("dve", "scalar_tensor_tensor", reads=[psB[bm], B_xres[xr]], writes=[B_acc[c]], out=accT[:, c, jr],
                       in0=xres[xr][:, jr], scalar=ALPHA, in1=ps[bm][:], op0=ALU.mult, op1=ALU.add)
            ws_done()
        p.barrier()

        def ln_feat(gi, bi, bf_out, B_bf):
            sqt = [tmp.alloc([128, 512], F32) for _ in range(2)]
            B_sqt = [Buf("sqt0"), Buf("sqt1")]
            mean_s = [tmp.alloc([128, 512], F32) for _ in range(2)]
            rstd = [tmp.alloc([128, 512], F32) for _ in range(2)]
            msq = tmp.alloc([128, 512], F32)
            tn = [tmp.alloc([128, 512], F32) for _ in range(2)]
            B_tn = [Buf("tn0"), Buf("tn1")]
            B_st = [Buf("lnstat0"), Buf("lnstat1")]
            B_msq = Buf("msq")
            for j in range(2):
                jr = slice(j * 512, (j + 1) * 512)
                for c in range(16):
                    MM(j, ones_f[:], accT[:, c, jr], c == 0, c == 15, [B_const, B_acc[c]])
                for c in range(16):
                    OP("act", "activation", reads=[B_acc[c]], writes=[B_sqt[c % 2]], out=sqt[c % 2][:], in_=accT[:, c, jr],
                       func=AF.Square)
                    MM(2 + j, ones_f[:], sqt[c % 2][:], c == 0, c == 15, [B_const, B_sqt[c % 2]])
            for j in range(2):
                OP("act", "activation", reads=[psB[j]], writes=[B_st[j]], out=mean_s[j][:], in_=ps[j][:], func=AF.Copy)
                OP("act", "activation", reads=[B_st[j]], writes=[B_msq], out=msq[:], in_=mean_s[j][:], func=AF.Square)
                OP("dve", "tensor_tensor", reads=[psB[2 + j], B_msq], writes=[B_st[j]], out=rstd[j][:], in0=ps[2 + j][:],
                   in1=msq[:], op=ALU.subtract)
                OP("act", "activation", reads=[B_st[j], B_const], writes=[B_st[j]], out=rstd[j][:], in_=rstd[j][:], func=AF.Sqrt,
                   bias=eps_ln[:], scale=1.0)
                OP("dve", "reciprocal", reads=[B_st[j]], writes=[B_st[j]], out=rstd[j][:], in_=rstd[j][:])
            for j in range(2):
                jr = slice(j * 512, (j + 1) * 512)
                for c in range(16):
                    ti = c % 2
                    OP("dve", "tensor_tensor", reads=[B_acc[c], B_st[j]], writes=[B_tn[ti]], out=tn[ti][:], in0=accT[:, c, jr],
                       in1=mean_s[j][:], op=ALU.subtract)
                    OP("pool" if ti == 0 else "dve", "tensor_tensor", reads=[B_tn[ti], B_st[j]], writes=[B_tn[ti]],
                       out=tn[ti][:], in0=tn[ti][:], in1=rstd[j][:], op=ALU.mult)
                    OP("act", "activation", reads=[B_tn[ti], B_const], writes=[B_acc[c]], out=accT[:, c, jr], in_=tn[ti][:],
                       func=AF.Identity, bias=vcol(bi, c), scale=vcol(gi, c))
                    if bf_out is not None:
                        OP("act", "activation", reads=[B_acc[c]], writes=[B_bf], out=bf_out[:, c, jr], in_=accT[:, c, jr],
                           func=AF.Copy)

        tmp.reset(r6_lo, SB_END)
        ln_feat(V_L1G, V_L1B, x1T, B_r3)
        dbg_dump("x1Tf", accT[:], B_acc)
        p.barrier()
        if stop == "E":
            return finish()

        tmp.reset(r6_lo, SB_END)
        sel = tmp.alloc([16, 16, 128], F32)
        combT = tmp.alloc([16, TOK], F32)
        B_sel, B_combT = Buf("sel"), Buf("combT")
        f_mark = tmp.off
        wr = tmp.alloc([128, 16, 20], F32)
        br = tmp.alloc([128, 20], F32)
        B_wr = Buf("wr")
        DMA("sp", p.dsem("wr"), [(wr[:], wr_d), (br[:], br_d), (sel[:], sel_d)], writes=[B_wr, B_sel])
        lg = tmp.alloc([128, 8, 20], F32)
        R = {k: tmp.alloc([128, 8, n], F32) for k, n in
             [("gmax", 1), ("gsh", 4), ("ge", 4), ("gsum", 1), ("gpt", 1), ("ohg", 4), ("els", 4), ("t4", 4), ("m1", 1),
              ("mask1", 4), ("els2", 4), ("m2", 1), ("mask2", 4), ("selm", 4), ("esh", 4), ("ee", 4), ("esum", 1),
              ("er", 1), ("ew", 4), ("gw", 4)]}
        comb = tmp.alloc([128, 8, 16], F32)
        B_rt_ = Buf("router")
        for s8 in range(8):
            for c in range(16):
                MM(0, accT[:, c, s8 * 128:(s8 + 1) * 128], wr[:, c, :], c == 0, c == 15, [B_acc[c], B_wr],
                   cols=(s8 * 20, (s8 + 1) * 20))

        def RO(meth, **kw):
            OP("dve", meth, reads=[B_rt_], writes=[B_rt_], **kw)

        def bc(ap, n):
            return ap.broadcast_to([128, 8, n])

        OP("dve", "tensor_tensor", reads=[psB[0], B_wr], writes=[B_rt_], out=lg[:],
           in0=ps[0][:, 0:160].rearrange("p (s n) -> p s n", n=20), in1=br[:].unsqueeze(1).broadcast_to([128, 8, 20]),
           op=ALU.add)
        gl = lg[:, :, 0:4]
        AXX = mybir.AxisListType.X
        RO("tensor_reduce", out=R["gmax"][:], in_=gl, axis=AXX, op=ALU.max)
        RO("tensor_tensor", out=R["gsh"][:], in0=gl, in1=bc(R["gmax"][:], 4), op=ALU.subtract)
        OP("act", "activation", reads=[B_rt_], writes=[B_rt_], out=R["ge"][:], in_=R["gsh"][:], func=AF.Exp)
        RO("tensor_reduce", out=R["gsum"][:], in_=R["ge"][:], axis=AXX, op=ALU.add)
        RO("reciprocal", out=R["gpt"][:], in_=R["gsum"][:])
        RO("tensor_tensor", out=R["ohg"][:], in0=gl, in1=bc(R["gmax"][:], 4), op=ALU.is_equal)
        for g in range(4):
            elg = lg[:, :, 4 + 4 * g:8 + 4 * g]
            if g == 0:
                RO("tensor_tensor", out=R["els"][:], in0=elg, in1=bc(R["ohg"][:, :, 0:1], 4), op=ALU.mult)
            else:
                RO("tensor_tensor", out=R["t4"][:], in0=elg, in1=bc(R["ohg"][:, :, g:g + 1], 4), op=ALU.mult)
                RO("tensor_tensor", out=R["els"][:], in0=R["els"][:], in1=R["t4"][:], op=ALU.add)
        RO("tensor_reduce", out=R["m1"][:], in_=R["els"][:], axis=AXX, op=ALU.max)
        RO("tensor_tensor", out=R["mask1"][:], in0=R["els"][:], in1=bc(R["m1"][:], 4), op=ALU.is_equal)
        RO("scalar_tensor_tensor", out=R["els2"][:], in0=R["mask1"][:], scalar=-1.0e30, in1=R["els"][:], op0=ALU.mult,
           op1=ALU.add)
        RO("tensor_reduce", out=R["m2"][:], in_=R["els2"][:], axis=AXX, op=ALU.max)
        RO("tensor_tensor", out=R["mask2"][:], in0=R["els2"][:], in1=bc(R["m2"][:], 4), op=ALU.is_equal)
        RO("tensor_tensor", out=R["selm"][:], in0=R["mask1"][:], in1=R["mask2"][:], op=ALU.add)
        RO("tensor_tensor", out=R["esh"][:], in0=R["els"][:], in1=bc(R["m1"][:], 4), op=ALU.subtract)
        OP("act", "activation", reads=[B_rt_], writes=[B_rt_], out=R["ee"][:], in_=R["esh"][:], func=AF.Exp)
        RO("tensor_tensor", out=R["ee"][:], in0=R["ee"][:], in1=R["selm"][:], op=ALU.mult)
        RO("tensor_reduce", out=R["esum"][:], in_=R["ee"][:], axis=AXX, op=ALU.add)
        RO("reciprocal", out=R["er"][:], in_=R["esum"][:])
        RO("tensor_tensor", out=R["ew"][:], in0=R["ee"][:], in1=bc(R["er"][:], 4), op=ALU.mult)
        RO("tensor_tensor", out=R["gw"][:], in0=R["ohg"][:], in1=bc(R["gpt"][:], 4), op=ALU.mult)
        RO("tensor_tensor", out=comb[:].rearrange("p s (g e) -> p s g e", g=4),
           in0=R["gw"][:].unsqueeze(3).broadcast_to([128, 8, 4, 4]),
           in1=R["ew"][:].unsqueeze(2).broadcast_to([128, 8, 4, 4]), op=ALU.mult)
        dbg_dump("comb", comb[:], [B_rt_])
        dbg_dump("logits", lg[:], [B_rt_])
        for s8 in range(8):
            bank = 1 + s8 // 4
            OP("pe", "transpose", reads=[B_rt_, B_const], writes=[psB[bank]],
               out=ps[bank][0:16, (s8 % 4) * 128:(s8 % 4 + 1) * 128], in_=comb[:, s8, :], identity=ident_f[:])
        for hf in range(2):
            OP("act", "activation", reads=[psB[1 + hf]], writes=[B_combT], out=combT[:, hf * 512:(hf + 1) * 512],
               in_=ps[1 + hf][0:16, :], func=AF.Copy)
        aux.reset()
        wple = aux.alloc([128, 2, D], BF16)
        pTs = aux.alloc([128, 2, TOK], BF16)
        B_ple = Buf("ple_in")
        DMA("pool", p.dsem("ple"), [(wple[:], w_ple.rearrange("(k p) n -> p k n", p=128)),
                                    (pTs[:], pT.rearrange("(k p) t -> p k t", p=128))], writes=[B_ple])
        sigt = [tmp.alloc([128, 512], F32) for _ in range(2)]
        tt = [tmp.alloc([128, 512], F32) for _ in range(2)]
        B_sig = [Buf("sig0"), Buf("sig1")]
        B_tt = [Buf("tt0"), Buf("tt1")]
        it = 0
        for sg in range(8):
            spg, B_spg = ws_next()
            for cc2 in range(2):
                c = sg * 2 + cc2
                cs = slice(cc2 * 128, (cc2 + 1) * 128)
                for j in range(2):
                    jr = slice(j * 512, (j + 1) * 512)
                    bg, bl, si = 3 + it % 2, 5 + it % 2, it % 2
                    it += 1
                    for k in range(16):
                        MM(bg, spg[:, k, cs], x1T[:, k, jr], k == 0, k == 15, [B_spg, B_r3])
                    for k in range(2):
                        MM(bl, wple[:, k, c * 128:(c + 1) * 128], pTs[:, k, jr], k == 0, k == 1, [B_ple])
                    OP("act", "activation", reads=[psB[bg], B_const], writes=[B_sig[si]], out=sigt[si][:], in_=ps[bg][:],
                       func=AF.Sigmoid, bias=vcol(V_BPG, c), scale=1.0)
                    OP("dve", "tensor_tensor", reads=[psB[bl], B_sig[si]], writes=[B_tt[si]], out=tt[si][:], in0=ps[bl][:],
                       in1=sigt[si][:], op=ALU.mult)
                    OP("dve", "scalar_tensor_tensor", reads=[B_acc[c], B_tt[si]], writes=[B_acc[c]], out=accT[:, c, jr],
                       in0=accT[:, c, jr], scalar=ALPHA, in1=tt[si][:], op0=ALU.mult, op1=ALU.add)
            ws_done()
        dbg_dump("acc0", accT[:], B_acc)
        p.barrier()
        if stop == "F":
            return finish()

        tmp.reset(f_mark, SB_END)
        aux.reset()
        hT = [aux.alloc([128, 8, TOK], BF16) for _ in range(2)]
        B_hT = [Buf("hT0"), Buf("hT1")]
        cbc = [tmp.alloc([128, TOK], F32) for _ in range(2)]
        B_cbc = [Buf("cbc0"), Buf("cbc1")]
        sgt = [tmp.alloc([128, 512], F32) for _ in range(2)]
        tt = [tmp.alloc([128, 512], F32) for _ in range(2)]
        B_sgt = [Buf("sgt0"), Buf("sgt1")]
        B_tt = [Buf("tt0"), Buf("tt1")]
        it = 0
        ity = 0
        for e in range(n_exp):
            hb = e % 2
            for j in range(2):
                MM(6, sel[:, e, :], combT[:, j * 512:(j + 1) * 512], True, True, [B_sel, B_combT])
                OP("act", "activation", reads=[psB[6]], writes=[B_cbc[hb]], out=cbc[hb][:, j * 512:(j + 1) * 512],
                   in_=ps[6][:], func=AF.Copy)
            for fq in range(4):
                s1, B_s1 = ws_next()
                s3, B_s3 = ws_next()
                for f2 in range(2):
                    fc = fq * 2 + f2
                    cs = slice(f2 * 128, (f2 + 1) * 128)
                    for j in range(2):
                        jr = slice(j * 512, (j + 1) * 512)
                        bg, bu, si = it % 2, 2 + it % 2, it % 2
                        it += 1
                        for k in range(16):
                            MM(bg, s1[:, k, cs], x1T[:, k, jr], k == 0, k == 15, [B_s1, B_r3])
                        for k in range(16):
                            MM(bu, s3[:, k, cs], x1T[:, k, jr], k == 0, k == 15, [B_s3, B_r3])
                        OP("act", "activation", reads=[psB[bg]], writes=[B_sgt[si]], out=sgt[si][:], in_=ps[bg][:],
                           func=AF.Silu)
                        OP("dve", "tensor_tensor", reads=[psB[bu], B_sgt[si]], writes=[B_tt[si]], out=tt[si][:],
                           in0=ps[bu][:], in1=sgt[si][:], op=ALU.mult)
                        OP("pool", "tensor_tensor", reads=[B_tt[si], B_cbc[hb]], writes=[B_hT[hb]], out=hT[hb][:, fc, jr],
                           in0=tt[si][:], in1=cbc[hb][:, jr], op=ALU.mult)
                ws_done(2)
            for cs4 in range(4):
                s2, B_s2 = ws_next()
                for cc in range(4):
                    c = cs4 * 4 + cc
                    for j in range(2):
                        jr = slice(j * 512, (j + 1) * 512)
                        by = 4 + ity % 2
                        ity += 1
                        for fc in range(8):
                            MM(by, s2[:, fc, cc * 128:(cc + 1) * 128], hT[hb][:, fc, jr], fc == 0, fc == 7, [B_s2, B_hT[hb]])
                        OP("dve", "tensor_tensor", reads=[psB[by], B_acc[c]], writes=[B_acc[c]], out=accT[:, c, jr],
                           in0=ps[by][:], in1=accT[:, c, jr], op=ALU.add)
                ws_done()
        p.barrier()

        tmp.reset(r6_lo, SB_END)
        ln_feat(V_L2G, V_L2B, None, None)
        p.barrier()
        aux.reset(r2_lo, r4_lo)
        osb = [aux.alloc([128, D], F32) for _ in range(2)]
        B_osb = [Buf("osb0"), Buf("osb1")]
        d_os = [p.dsem("osb0"), p.dsem("osb1")]
        for s8 in range(8):
            ob = s8 % 2
            for cq in range(4):
                bank = cq % 2
                for c4 in range(4):
                    c = cq * 4 + c4
                    OP("pe", "transpose", reads=[B_acc[c], B_const], writes=[psB[bank]],
                       out=ps[bank][:, c4 * 128:(c4 + 1) * 128], in_=accT[:, c, s8 * 128:(s8 + 1) * 128], identity=ident_f[:])
                if bank == 0:
                    OP("act", "activation", reads=[psB[bank]], writes=[B_osb[ob]], out=osb[ob][:, cq * 512:(cq + 1) * 512],
                       in_=ps[bank][:], func=AF.Copy)
                else:
                    OP("dve", "tensor_copy", reads=[psB[bank]], writes=[B_osb[ob]], out=osb[ob][:, cq * 512:(cq + 1) * 512],
                       in_=ps[bank][:])
            DMA("sp", d_os[ob], [(out[s8 * 128:(s8 + 1) * 128, :], osb[ob][:])], reads=[B_osb[ob]])
        return finish()


def _rope_tables():
    half = 64
    inv_freq = (1.0 / (np.float32(10000.0) ** (np.arange(0, half, 2, dtype=np.float32) / np.float32(half)))).astype(np.float32)
    tok = np.arange(S)
    row = (tok // 64).astype(np.float32)
    col = (tok % 64).astype(np.float32)
    ang_r = (row[:, None] * inv_freq[None, :]).astype(np.float32)
    ang_c = (col[:, None] * inv_freq[None, :]).astype(np.float32)
    cos = np.concatenate([np.cos(ang_r), np.cos(ang_r), np.cos(ang_c), np.cos(ang_c)], axis=1).astype(np.float32)
    sin = np.concatenate([-np.sin(ang_r), np.sin(ang_r), -np.sin(ang_c), np.sin(ang_c)], axis=1).astype(np.float32)
    return np.ascontiguousarray(cos.T), np.ascontiguousarray(sin.T)


def _pc(v):
    return np.ascontiguousarray(np.asarray(v, np.float32).reshape(16, 128).T)


def prep_inputs(inp, cores=range(NCORES)):
    f = lambda a: np.ascontiguousarray(np.asarray(a, np.float32))
    x = f(inp["x"][0])
    pfull = f(inp["p"][0, 0])
    cosT, sinT = _rope_tables()
    vecs = np.stack([_pc(inp["emb_ln_g"]), _pc(inp["emb_ln_b"]), _pc(inp["ln1_g"][0]), _pc(inp["ln1_b"][0]),
                     _pc(inp["ln2_g"][0]), _pc(inp["ln2_b"][0]), _pc(inp["b_gate"][0, :D]), _pc(inp["b_gate"][0, D:]),
                     _pc(inp["b_ple_gate"][0]), _pc(inp["conv_w"][0, 0]), _pc(inp["conv_w"][0, 1]), _pc(inp["conv_w"][0, 2])],
                    axis=1)
    vecs = np.ascontiguousarray(vecs.astype(np.float32))
    ident = np.eye(128, dtype=np.float32)
    rmat = np.zeros((128, 128), np.float32)
    for m in range(128):
        partner = m + 32 if (m % 64) < 32 else m - 32
        rmat[partner, m] = 1.0
    cmat = np.ascontiguousarray(np.stack([ident, rmat], axis=1))
    gq = f(inp["q_norm_g"][0]); gk = f(inp["k_norm_g"][0])
    gqk = np.ascontiguousarray(np.stack([gq, gk], axis=1))
    gqk_row = np.ascontiguousarray(np.broadcast_to(np.stack([gq, gk], axis=0)[None], (128, 2, 128)))
    wr = np.concatenate([f(inp["w_group_router"][0]), f(inp["w_expert_router"][0]).reshape(D, 16)], axis=1)
    wr = np.ascontiguousarray(wr.reshape(16, 128, 20).transpose(1, 0, 2))
    br = np.concatenate([f(inp["b_group_router"][0]), f(inp["b_expert_router"][0]).reshape(16)])
    br = np.ascontiguousarray(np.broadcast_to(br[None], (128, 20)))
    sel = np.zeros((16, 16, 128), np.float32)
    for e in range(16):
        sel[e, e, :] = 1.0
    shared = {
        "x_all": x, "cos_all": cosT, "sin_all": sinT,
        "w_in": f(inp["w_in"][0]), "w_ao": f(inp["w_attn_o"][0]), "w_co": f(inp["w_conv_o"][0]),
        "w_out": f(inp["w_out"][0]), "w_pg": f(inp["w_ple_gate"][0]), "w_ple": f(inp["w_ple"][0]),
        "w1": f(inp["w_exp_gate"][0]).reshape(16, D, 1024), "w3": f(inp["w_exp_up"][0]).reshape(16, D, 1024),
        "w2": f(inp["w_exp_down"][0]).reshape(16, 1024, D),
        "vecs": vecs, "cmat": cmat, "gqk": gqk, "gqk_row": gqk_row, "wr": wr, "br": br, "sel": sel,
    }
    maps = []
    for c in cores:
        lo, hi = c * TOK, (c + 1) * TOK
        halo = np.zeros((2, D), np.float32)
        hm = np.zeros((128, 2), np.float32)
        if lo > 0:
            halo[0] = x[lo - 1]; hm[:, 0] = 1.0
        if hi < S:
            halo[1] = x[hi]; hm[:, 1] = 1.0
        m = dict(shared)
        m.update({"x_own": np.ascontiguousarray(x[lo:hi]), "x_halo": halo, "hmask": hm,
                  "pT": np.ascontiguousarray(pfull[lo:hi].T),
                  "cos_own": np.ascontiguousarray(cosT[:, lo:hi]), "sin_own": np.ascontiguousarray(sinT[:, lo:hi])})
        maps.append(m)
    return maps


def kernel(**inp):
    nc = build()
    maps = prep_inputs(inp)
    res = run_bass_kernel_spmd(nc, maps, core_ids=list(range(NCORES)))
    outs = [np.asarray(r["out"], np.float32) for r in res.results]
    return np.concatenate(outs, axis=0).reshape(1, S, D)
```
